# Optimizing a Trainium2 kernel written in Bass

```python
import math
import jax, jax.numpy as jnp
from jax import lax
import numpy as np

D_MODEL = 1024
BATCH = 8
SEQ = 8192
DEPTH = 1

ATTN_HEADS = D_MODEL // 256
ATTN_HEAD_DIM = 64
ATTN_V_DIM = 2 * ATTN_HEAD_DIM
ATTN_QK_WIDTH = ATTN_HEADS * 2 * ATTN_HEAD_DIM
ATTN_WIDTH = ATTN_HEADS * ATTN_V_DIM
Q_BLOCK = 128
REC_HEADS = D_MODEL // 256
REC_KEY_DIM = 128
REC_VAL_DIM = 128
REC_KEY_WIDTH = REC_HEADS * REC_KEY_DIM
REC_WIDTH = REC_HEADS * REC_VAL_DIM
REC_CHUNK = 64
IN_SPLITS = (ATTN_QK_WIDTH, ATTN_QK_WIDTH, ATTN_WIDTH, REC_KEY_WIDTH, REC_KEY_WIDTH, REC_WIDTH, REC_WIDTH)
IN_WIDTH = sum(IN_SPLITS)
MIX_WIDTH = ATTN_WIDTH + REC_WIDTH
NUM_BUCKETS = 32
MAX_DISTANCE = 128
N_GROUPS = 4
EXPERTS_PER_GROUP = 8
N_EXPERTS = N_GROUPS * EXPERTS_PER_GROUP
EXPERT_FF = D_MODEL // 2
TOP_K = 2
EXPERT_BLOCK = 128
EPS = 1e-6

kernel_name = 'hybrid_diffattn_hgrn2_hier_moe'


def rmsnorm(x, g):
    xf = x.astype(jnp.float32)
    y = xf * lax.rsqrt(jnp.mean(xf * xf, axis=-1, keepdims=True) + EPS)
    return (y * g.astype(jnp.float32)).astype(x.dtype)


def t5_bucket(rel):
    n = jnp.maximum(rel, 0)
    max_exact = NUM_BUCKETS // 2
    nf = jnp.maximum(n, 1).astype(jnp.float32)
    large = max_exact + (jnp.log(nf / max_exact) / math.log(MAX_DISTANCE / max_exact)
                         * (NUM_BUCKETS - max_exact)).astype(jnp.int32)
    large = jnp.minimum(large, NUM_BUCKETS - 1)
    return jnp.where(n < max_exact, n, large)


def lambda_init(layer):
    return 0.8 - 0.6 * math.exp(-0.3 * layer)


def diff_attention(q, k, v, lq1, lk1, lq2, lk2, subln_g, rel_table, layer):
    B, S, _ = q.shape
    nblk = S // Q_BLOCK
    q = q.reshape(B, S, ATTN_HEADS, 2, ATTN_HEAD_DIM).transpose(0, 2, 3, 1, 4)
    k = k.reshape(B, S, ATTN_HEADS, 2, ATTN_HEAD_DIM).transpose(0, 2, 3, 1, 4)
    v = v.reshape(B, S, ATTN_HEADS, ATTN_V_DIM).transpose(0, 2, 1, 3)
    lam_init = lambda_init(layer)
    f32 = jnp.float32
    lam = (jnp.exp(jnp.sum(lq1.astype(f32) * lk1.astype(f32)))
           - jnp.exp(jnp.sum(lq2.astype(f32) * lk2.astype(f32))) + lam_init)
    scale = ATTN_HEAD_DIM ** -0.5
    q_blocks = jnp.moveaxis(q.reshape(B, ATTN_HEADS, 2, nblk, Q_BLOCK, ATTN_HEAD_DIM), 3, 0)
    k_pos = jnp.arange(S)

    def block(args):
        qb, start = args
        q_pos = start + jnp.arange(Q_BLOCK)
        rel = q_pos[:, None] - k_pos[None, :]
        bias = jnp.transpose(rel_table[t5_bucket(rel)], (2, 0, 1)).astype(f32)
        logits = jnp.einsum('bhcqd,bhckd->bhcqk', qb, k).astype(f32) * scale + bias[None, :, None]
        logits = jnp.where(rel >= 0, logits, -jnp.inf)
        p = jax.nn.softmax(logits, axis=-1)
        a = p[:, :, 0] - lam * p[:, :, 1]
        return jnp.einsum('bhqk,bhkv->bhqv', a.astype(v.dtype), v)

    o = lax.map(block, (q_blocks, jnp.arange(nblk) * Q_BLOCK))
    o = jnp.moveaxis(o, 0, 2).reshape(B, ATTN_HEADS, S, ATTN_V_DIM).transpose(0, 2, 1, 3)
    o = rmsnorm(o, subln_g) * (1.0 - lam_init)
    return o.reshape(B, S, ATTN_WIDTH)


def hgrn2(q, f, i, g, lb, norm_g):
    B, S, _ = q.shape
    f32 = jnp.float32
    n = S // REC_CHUNK
    qf = jax.nn.silu(q.astype(f32))
    fr = f.astype(f32)
    lb = lb.astype(f32)
    log_f = jnp.log(lb + (1.0 - lb) * jax.nn.sigmoid(fr))
    kf = (1.0 - lb) * jax.nn.sigmoid(-fr)
    vf = i.astype(f32)

    def to_chunks(t, d):
        return t.reshape(B, n, REC_CHUNK, REC_HEADS, d).transpose(1, 0, 3, 2, 4)

    qc = to_chunks(qf, REC_KEY_DIM)
    kc = to_chunks(kf, REC_KEY_DIM)
    fc = to_chunks(log_f, REC_KEY_DIM)
    vc = to_chunks(vf, REC_VAL_DIM)
    tri = jnp.tril(jnp.ones((REC_CHUNK, REC_CHUNK), bool))

    def step(state, inp):
        qt, kt, lft, vt = inp
        b = jnp.cumsum(lft, axis=2)
        o_inter = jnp.einsum('bhtk,bhkv->bhtv', qt * jnp.exp(b), state)
        rel_decay = jnp.exp(jnp.where(tri[:, :, None],
                                      b[:, :, :, None, :] - b[:, :, None, :, :], -jnp.inf))
        scores = jnp.einsum('bhtk,bhsk,bhtsk->bhts', qt, kt, rel_decay)
        o = o_inter + jnp.einsum('bhts,bhsv->bhtv', scores, vt)
        b_last = b[:, :, -1:, :]
        state = (jnp.exp(b_last[:, :, 0, :])[..., None] * state
                 + jnp.einsum('bhsk,bhsv->bhkv', kt * jnp.exp(b_last - b), vt))
        return state, o

    s0 = jnp.zeros((B, REC_HEADS, REC_KEY_DIM, REC_VAL_DIM), f32)
    _, o = lax.scan(step, s0, (qc, kc, fc, vc))
    o = o.transpose(1, 0, 3, 2, 4).reshape(B, S, REC_HEADS, REC_VAL_DIM)
    o = rmsnorm(o, norm_g.reshape(REC_HEADS, REC_VAL_DIM))
    o = o * jax.nn.silu(g.astype(f32)).reshape(B, S, REC_HEADS, REC_VAL_DIM)
    return o.reshape(B, S, REC_WIDTH).astype(g.dtype)


def hier_moe(h, w_group, b_group, w_expert, b_expert, w1, w3, w2):
    B, S, D = h.shape
    N = B * S
    A = N * TOP_K
    f32 = jnp.float32
    hf = h.reshape(N, D)
    g_prob = jax.nn.softmax((hf @ w_group + b_group).astype(f32), axis=-1)
    g_gate, g_idx = lax.top_k(g_prob, 1)
    e_logits = (hf @ w_expert + b_expert).astype(f32).reshape(N, N_GROUPS, EXPERTS_PER_GROUP)
    e_logits = jnp.take_along_axis(e_logits, g_idx[:, :, None], axis=1)[:, 0]
    e_top, e_idx = lax.top_k(jax.nn.softmax(e_logits, axis=-1), TOP_K)
    weights = g_gate * e_top / jnp.sum(e_top, axis=-1, keepdims=True)
    expert_id = (g_idx * EXPERTS_PER_GROUP + e_idx).reshape(A)
    order = jnp.argsort(expert_id)
    sorted_e = expert_id[order]
    tok = order // TOP_K
    w_sorted = weights.reshape(A)[order]
    counts = jnp.bincount(expert_id, length=N_EXPERTS)
    start = jnp.cumsum(counts) - counts
    padded = (counts + EXPERT_BLOCK - 1) // EXPERT_BLOCK * EXPERT_BLOCK
    padded_end = jnp.cumsum(padded)
    padded_start = padded_end - padded
    dest = padded_start[sorted_e] + jnp.arange(A) - start[sorted_e]
    n_blocks = -(-A // EXPERT_BLOCK) + N_EXPERTS
    rows = jnp.zeros((n_blocks * EXPERT_BLOCK, D), h.dtype).at[dest].set(hf[tok])
    block_e = jnp.minimum(jnp.searchsorted(padded_end, jnp.arange(n_blocks) * EXPERT_BLOCK, side='right'),
                          N_EXPERTS - 1)

    def expert_block(args):
        xb, e = args
        return (jax.nn.silu(xb @ w1[e]) * (xb @ w3[e])) @ w2[e]

    ys = lax.map(expert_block, (rows.reshape(n_blocks, EXPERT_BLOCK, D), block_e)).reshape(-1, D)
    y = ys[dest] * w_sorted[:, None].astype(h.dtype)
    out = jnp.zeros((N, D), h.dtype).at[tok].add(y)
    return out.reshape(B, S, D)


def _normal(k, shape, s):
    return jax.random.normal(k, shape, jnp.float32) * s


def setup_inputs(seed: int = 0) -> dict:
    key = jax.random.key(seed)
    ks = jax.random.split(key, 24)
    D = D_MODEL
    return {
        'x': _normal(ks[0], (BATCH, SEQ, D), 1.0),
        'c': _normal(ks[1], (BATCH, D), 1.0),
        'w_ada': _normal(ks[2], (DEPTH, D, 6 * D), D ** -0.5),
        'b_ada': _normal(ks[3], (DEPTH, 6 * D), 0.02),
        'norm1_g': 1.0 + _normal(ks[4], (DEPTH, D), 0.02),
        'norm2_g': 1.0 + _normal(ks[5], (DEPTH, D), 0.02),
        'w_in': _normal(ks[6], (DEPTH, D, IN_WIDTH), D ** -0.5),
        'attn_lambda_q1': _normal(ks[7], (DEPTH, ATTN_HEAD_DIM), 0.1),
        'attn_lambda_k1': _normal(ks[8], (DEPTH, ATTN_HEAD_DIM), 0.1),
        'attn_lambda_q2': _normal(ks[9], (DEPTH, ATTN_HEAD_DIM), 0.1),
        'attn_lambda_k2': _normal(ks[10], (DEPTH, ATTN_HEAD_DIM), 0.1),
        'attn_subln_g': 1.0 + _normal(ks[11], (DEPTH, ATTN_V_DIM), 0.02),
        'rel_bias_table': _normal(ks[12], (NUM_BUCKETS, ATTN_HEADS), 0.5),
        'rec_lower_bound': _normal(ks[13], (DEPTH + 1, REC_KEY_WIDTH), 0.1),
        'rec_norm_g': 1.0 + _normal(ks[14], (DEPTH, REC_WIDTH), 0.02),
        'w_out': _normal(ks[15], (DEPTH, MIX_WIDTH, D), MIX_WIDTH ** -0.5),
        'w_group': _normal(ks[16], (DEPTH, D, N_GROUPS), D ** -0.5),
        'b_group': _normal(ks[17], (DEPTH, N_GROUPS), 0.01),
        'w_expert': _normal(ks[18], (DEPTH, D, N_EXPERTS), D ** -0.5),
        'b_expert': _normal(ks[19], (DEPTH, N_EXPERTS), 0.01),
        'w1': _normal(ks[20], (DEPTH, N_EXPERTS, D, EXPERT_FF), D ** -0.5),
        'w3': _normal(ks[21], (DEPTH, N_EXPERTS, D, EXPERT_FF), D ** -0.5),
        'w2': _normal(ks[22], (DEPTH, N_EXPERTS, EXPERT_FF, D), EXPERT_FF ** -0.5),
        'final_norm_g': 1.0 + _normal(ks[23], (D,), 0.02),
    }


def reference(x, c, w_ada, b_ada, norm1_g, norm2_g, w_in, attn_lambda_q1, attn_lambda_k1,
              attn_lambda_q2, attn_lambda_k2, attn_subln_g, rel_bias_table, rec_lower_bound,
              rec_norm_g, w_out, w_group, b_group, w_expert, b_expert, w1, w3, w2, final_norm_g):
    lb_all = jnp.cumsum(jax.nn.softmax(rec_lower_bound.astype(jnp.float32), axis=0), axis=0)
    c_act = jax.nn.silu(c)
    split_points = []
    acc = 0
    for w in IN_SPLITS[:-1]:
        acc += w
        split_points.append(acc)
    for l in range(DEPTH):
        mod = c_act @ w_ada[l] + b_ada[l]
        sh1, sc1, g1, sh2, sc2, g2 = jnp.split(mod, 6, axis=-1)
        h = rmsnorm(x, norm1_g[l]) * (1.0 + sc1[:, None]) + sh1[:, None]
        proj = h @ w_in[l]
        qa, ka, va, qr, fr, ir, gr = jnp.split(proj, split_points, axis=-1)
        ya = diff_attention(qa, ka, va, attn_lambda_q1[l], attn_lambda_k1[l], attn_lambda_q2[l],
                            attn_lambda_k2[l], attn_subln_g[l], rel_bias_table, l)
        yr = hgrn2(qr, fr, ir, gr, lb_all[l], rec_norm_g[l])
        mix = jnp.concatenate([ya, yr], axis=-1) @ w_out[l]
        x = x + g1[:, None] * mix
        h = rmsnorm(x, norm2_g[l]) * (1.0 + sc2[:, None]) + sh2[:, None]
        x = x + g2[:, None] * hier_moe(h, w_group[l], b_group[l], w_expert[l], b_expert[l],
                                       w1[l], w3[l], w2[l])
    return rmsnorm(x, final_norm_g)
```

```python
import math
import threading
import numpy as np
import ml_dtypes
from contextlib import ExitStack
import concourse.bass as bass
import concourse.mybir as mybir
from concourse.bass_utils import run_bass_kernel_spmd

F32 = mybir.dt.float32
BF16 = mybir.dt.bfloat16
I32 = mybir.dt.int32
AF = mybir.ActivationFunctionType
ALU = mybir.AluOpType
AX = mybir.AxisListType


class Buf:
    __slots__ = ("w", "r")

    def __init__(self):
        self.w = {}
        self.r = {}


class Ctx:
    NQ = 24

    def __init__(self, nc, st):
        self.nc = nc
        self.eng = {"pe": nc.tensor, "act": nc.scalar, "dve": nc.vector, "pool": nc.gpsimd, "sp": nc.sync}
        self.csem = {k: st.enter_context(nc.semaphore("c_" + k)) for k in ("pe", "act", "dve", "pool")}
        self.ccnt = {k: 0 for k in self.csem}
        self.dsem = {q: [st.enter_context(nc.semaphore("d_%s%d" % (q, i))) for i in range(self.NQ)]
                     for q in ("sp", "act", "pool")}
        self.dcnt = {q: [0] * self.NQ for q in self.dsem}
        self.drr = {q: 0 for q in self.dsem}
        self.known = {e: {} for e in self.eng}
        self._workers = {}

    class _Worker:
        def __init__(self, fn):
            self.fn = fn
            self.go = threading.Semaphore(0)
            self.back = threading.Semaphore(0)
            self.done = False
            self.exc = None
            self.th = threading.Thread(target=self._run, daemon=True)

        def _run(self):
            self.go.acquire()
            try:
                self.fn()
            except BaseException as e:
                self.exc = e
            self.done = True
            self.back.release()

        def gate(self):
            self.back.release()
            self.go.acquire()

        def step(self):
            self.go.release()
            self.back.acquire()

    def zip_emit(self, fns, weights=None):
        ws = [Ctx._Worker(f) for f in fns]
        if weights is None:
            weights = [1] * len(ws)
        for w in ws:
            self._workers[w.th] = w
            w.th.start()
        while not all(w.done for w in ws):
            for w, k in zip(ws, weights):
                for _ in range(k):
                    if not w.done:
                        w.step()
        for w in ws:
            del self._workers[w.th]
            if w.exc is not None:
                raise w.exc

    def _gate(self):
        w = self._workers.get(threading.current_thread())
        if w is not None:
            w.gate()

    def _sem(self, key):
        if key[0] == "c":
            return self.csem[key[1]]
        return self.dsem[key[1]][key[2]]

    def _wait(self, en, evs):
        kn = self.known[en]
        for key, val in evs.items():
            if key == ("c", "pe") and en == "pe":
                continue
            if kn.get(key, 0) < val:
                self.eng[en].wait_ge(self._sem(key), val)
                kn[key] = val

    @staticmethod
    def _deps(reads, writes):
        evs = {}
        for b in reads:
            for k, v in b.w.items():
                if evs.get(k, 0) < v:
                    evs[k] = v
        for b in writes:
            for d in (b.w, b.r):
                for k, v in d.items():
                    if evs.get(k, 0) < v:
                        evs[k] = v
        return evs

    @staticmethod
    def _record(ev, reads, writes):
        k, v = ev
        for b in reads:
            b.r[k] = v
        for b in writes:
            b.w = {k: v}
            b.r = {}

    def op(self, en, fn, reads=(), writes=()):
        self._gate()
        self._wait(en, self._deps(reads, writes))
        inst = fn(self.eng[en])
        self.ccnt[en] += 1
        inst.then_inc(self.csem[en], 1)
        self._record((("c", en), self.ccnt[en]), reads, writes)
        return inst

    def _dma_common(self, q, reads, writes, issue):
        self._gate()
        i = self.drr[q]
        self.drr[q] = (i + 1) % self.NQ
        evs = self._deps(reads, writes)
        prev = self.dcnt[q][i]
        if prev > 0:
            evs[("d", q, i)] = prev
        self._wait(q, evs)
        inst = issue(self.eng[q])
        self.dcnt[q][i] += 16
        inst.then_inc(self.dsem[q][i], 16)
        self._record((("d", q, i), self.dcnt[q][i]), reads, writes)
        return inst

    def dma(self, q, out, in_, reads=(), writes=(), **kw):
        return self._dma_common(q, reads, writes, lambda e: e.dma_start(out=out, in_=in_, **kw))

    def idma(self, out, out_off, in_, in_off, reads=(), writes=(), bounds=None):
        def issue(e):
            oo = bass.IndirectOffsetOnAxis(ap=out_off, axis=0) if out_off is not None else None
            io = bass.IndirectOffsetOnAxis(ap=in_off, axis=0) if in_off is not None else None
            if bounds is not None:
                return e.indirect_dma_start(out=out, out_offset=oo, in_=in_, in_offset=io,
                                            bounds_check=bounds, oob_is_err=False)
            return e.indirect_dma_start(out=out, out_offset=oo, in_=in_, in_offset=io)
        return self._dma_common("pool", reads, writes, issue)

    def barrier(self):
        evs = {}
        for k, c in self.ccnt.items():
            if c > 0:
                evs[("c", k)] = c
        for q in self.dsem:
            for i, c in enumerate(self.dcnt[q]):
                if c > 0:
                    evs[("d", q, i)] = c
        for en in self.eng:
            kn = self.known[en]
            for key, val in evs.items():
                if kn.get(key, 0) < val:
                    self.eng[en].wait_ge(self._sem(key), val)
                    kn[key] = val


EPS = 1e-6
LAM_INIT = 0.8 - 0.6 * math.exp(-0.3 * 0)
BLK = 256


def _t5_bucket_np(rel):
    n = np.maximum(rel, 0)
    nf = np.maximum(n, 1).astype(np.float32)
    large = 16 + (np.log(nf / np.float32(16)) / np.float32(math.log(8.0)) * np.float32(16)).astype(np.int32)
    large = np.minimum(large, 31)
    return np.where(n < 16, n, large)


def _consts(NB):
    p = np.arange(128)[:, None]
    j = np.arange(128)[None, :]
    cols = []
    cols.append((p == j))
    cols.append((p <= j).astype(np.float32) - (p <= 63))
    cols.append((p > j))
    cols.append((p <= j))
    cols.append((p < j))
    cols.append(np.ones((128, 128)))
    rel0 = j - p
    cols.append(np.where(rel0 >= 0, _t5_bucket_np(rel0), -1))
    cols.append(_t5_bucket_np(128 + j - p))
    cols.append(np.arange(128)[:, None])
    cols.append(np.broadcast_to(np.arange(NB)[None, :] * BLK, (128, NB)))
    cols.append(np.concatenate([np.ones((128, 1)), (p <= 63)], axis=1))
    return np.concatenate([np.asarray(c, np.float32) for c in cols], axis=1)


def build(S, debug=False):
    NT = S // 128
    NJ = S // 512
    NB = (2 * S) // BLK + 32
    NROW = NB * BLK
    NC = 1025 + NB + 2
    nc = bass.Bass("TRN2", target_bir_lowering=False)

    def din(name, shape, dt=F32):
        return nc.dram_tensor(name, shape, dt, kind="ExternalInput").ap()

    x = din("x", [S, 1024])
    c_col = din("c_col", [128, 8])
    w_ada = din("w_ada", [1024, 6144])
    b_ada = din("b_ada", [1, 6144])
    n1g_col = din("n1g_col", [128, 8])
    n2g_rep = din("n2g_rep", [128, 1024])
    fng_rep = din("fng_rep", [128, 1024])
    w_in = din("w_in", [1024, 3584])
    lamv = din("lamv", [128, 256])
    sg_rep = din("sg_rep", [128, 128])
    tb_rep = din("tb_rep", [128, 128])
    rlb_rep = din("rlb_rep", [128, 1024])
    rng_rep = din("rng_rep", [128, 512])
    w_out = din("w_out", [1024, 1024])
    wr = din("wr", [128, 8, 36])
    br_rep = din("br_rep", [128, 36])
    w1r = din("w1r", [32 * 128, 4096])
    w3r = din("w3r", [32 * 128, 4096])
    w2r = din("w2r", [32 * 128, 4096])
    consts = din("consts", [128, NC])
    out = nc.dram_tensor("out", [S, 1024], F32, kind="ExternalOutput").ap()
    skind = "ExternalOutput" if debug else "Internal"

    def dscr(name, shape, dt):
        return nc.dram_tensor(name, shape, dt, kind=skind).ap()

    WP = dscr("WP", [1024, 3584], BF16)
    WOP = dscr("WOP", [1024, 1024], BF16)
    QT = dscr("QT", [4, 128, S], BF16)
    QR = dscr("QR", [S, 512], BF16)
    FR = dscr("FR", [S, 512], F32)
    IR = dscr("IR", [S, 512], BF16)
    GR = dscr("GR", [S, 512], BF16)
    MIX = dscr("MIX", [S, 1024], BF16)
    X1 = dscr("X1", [S, 1024], F32)
    H2 = dscr("H2", [S, 1024], BF16)
    HS = dscr("HS", [NROW, 1024], BF16)
    REPS = dscr("REPS", [3, 128, 1024], F32)
    WB = [dscr("W%dB" % i, [32 * 128, 4096], BF16) for i in range(3)]
    YS = dscr("YS", [NROW, 1024], BF16)
    if debug:
        DBG = nc.dram_tensor("DBG", [128, 4096], F32, kind="ExternalOutput").ap()

    kcp = lambda ap: ap.rearrange("(kc p) n -> p kc n", p=128)

    with ExitStack() as top:
        cx = Ctx(nc, top)
        wbound = nc.gpsimd.alloc_register("wbound")
        nc.gpsimd.reg_mov(wbound, 32 * 128 - 1)

        def TB(st, name, shape, dt):
            return st.enter_context(nc.sbuf_tensor(name, shape, dt)), Buf()

        def PB(st, name, shape, dt):
            return st.enter_context(nc.psum_tensor(name, shape, dt)), Buf()

        class Rot:
            def __init__(self, items):
                self.items = items
                self.i = 0

            def next(self):
                it = self.items[self.i % len(self.items)]
                self.i += 1
                return it

        cst, b_cst = TB(top, "cst", [128, NC], F32)
        ident_f = cst[:, 0:128]
        L1 = cst[:, 128:256]
        L2 = cst[:, 256:384]
        MT = cst[:, 384:512]
        US_f = cst[:, 512:640]
        ones_f = cst[:, 640:768]
        idxD = [cst[:, 768:896], cst[:, 896:1024]]
        piota = cst[:, 1024:1025]
        bpos = cst[:, 1025:1025 + NB]
        sel2 = cst[:, 1025 + NB:1027 + NB]
        cbf, b_cbf = TB(top, "cbf", [128, 384], BF16)
        ident_b = cbf[:, 0:128]
        US_b = cbf[:, 128:256]
        ones_b = cbf[:, 256:384]
        neglam, b_neglam = TB(top, "neglam", [128, 1], F32)
        negb31, b_negb31 = TB(top, "negb31", [128, 4], F32)
        tbr, b_tbr = TB(top, "tbr", [128, 128], F32)
        EB, b_EB = TB(top, "EB", [128, 8, 128], BF16)
        sgr, b_sgr = TB(top, "sgr", [128, 128], F32)
        biascol, b_biascol = TB(top, "biascol", [128, 8], F32)
        bias_hl, b_bias_hl = TB(top, "bias_hl", [33, 3584], BF16)
        WTS, b_WTS = TB(top, "WTS", [128, NT, 2], F32)
        DESTi, b_DESTi = TB(top, "DESTi", [128, NT, 2], I32)
        widx, b_widx = TB(top, "widx", [128, NB], I32)

        cx.dma("sp", cst[:], consts, writes=[b_cst])
        cx.dma("sp", tbr[:], tb_rep, writes=[b_tbr])
        cx.dma("sp", sgr[:], sg_rep, writes=[b_sgr])
        cx.op("dve", lambda e: e.tensor_copy(out=cbf[:, 0:128], in_=ident_f), reads=[b_cst], writes=[b_cbf])
        cx.op("dve", lambda e: e.tensor_copy(out=cbf[:, 128:256], in_=US_f), reads=[b_cst], writes=[b_cbf])
        cx.op("dve", lambda e: e.tensor_copy(out=cbf[:, 256:384], in_=ones_f), reads=[b_cst], writes=[b_cbf])

        mhalf, b_mhalf = TB(top, "mhalf", [128, 8], F32)
        cx.op("pool", lambda e: e.memset(mhalf[:], -0.5), writes=[b_mhalf])

        def rstd_from_ssq(ssq_ap, out_ap, tmp_ap, n, rbufs, wbufs, via="pool"):
            w = ssq_ap.shape[-1]
            cx.op("dve", lambda e: e.tensor_scalar(out=tmp_ap, in0=ssq_ap, scalar1=1.0 / n, scalar2=EPS,
                                                   op0=ALU.mult, op1=ALU.add), reads=rbufs, writes=wbufs)
            if via == "act":
                cx.op("act", lambda e: e.activation(out=tmp_ap, in_=tmp_ap, func=AF.Ln), reads=wbufs, writes=wbufs)
                cx.op("act", lambda e: e.activation(out=out_ap, in_=tmp_ap, func=AF.Exp, scale=-0.5),
                      reads=wbufs, writes=wbufs)
                return
            cx.op("pool", lambda e: e.tensor_tensor(out=out_ap, in0=tmp_ap, in1=mhalf[:, 0:w], op=ALU.pow),
                  reads=list(wbufs) + [b_mhalf], writes=wbufs)

        with ExitStack() as st:
            psb = [PB(st, "p0_%d" % i, [128, 512], F32) for i in range(4)]
            prot = Rot(psb)
            g2rep, b_g2rep = TB(st, "g2rep", [128, 1024], F32)
            a2rep, b_a2rep = TB(st, "a2rep", [128, 1024], F32)
            sh2rep, b_sh2rep = TB(st, "sh2rep", [128, 1024], F32)
            ccol, b_ccol = TB(st, "ccol", [128, 8], F32)
            cact, b_cact = TB(st, "cact", [128, 8], F32)
            badar, b_badar = TB(st, "badar", [1, 6144], F32)
            modr, b_modr = TB(st, "modr", [1, 6144], F32)
            n1gc, b_n1gc = TB(st, "n1gc", [128, 8], F32)
            sh1c, b_sh1c = TB(st, "sh1c", [128, 8], F32)
            s1c, b_s1c = TB(st, "s1c", [128, 8], F32)
            g1rep, b_g1rep = TB(st, "g1rep", [128, 1024], F32)
            n2gr, b_n2gr = TB(st, "n2gr", [128, 1024], F32)
            biasr, b_biasr = TB(st, "biasr", [33, 3584], F32)
            bt32, b_bt32 = TB(st, "bt32", [33, 3584], F32)
            bh32, b_bh32 = TB(st, "bh32", [33, 3584], BF16)
            sh1rep, b_sh1rep = TB(st, "sh1rep", [128, 8, 33], F32)
            wbig = [TB(st, "wbig%d" % i, [128, 8, 512], F32) for i in range(3)]
            wcast = [TB(st, "wcast%d" % i, [128, 8, 512], BF16) for i in range(3)]
            lv, b_lv = TB(st, "lv", [128, 256], F32)
            ltmp, b_ltmp = TB(st, "ltmp", [128, 128], F32)
            lsum, b_lsum = TB(st, "lsum", [128, 2], F32)
            eq, b_eq = TB(st, "eq", [128, 128], F32)
            bacc = [TB(st, "bacc%d" % h, [128, 128], F32) for h in range(4)]

            cx.dma("sp", ccol[:], c_col, writes=[b_ccol])
            cx.dma("sp", badar[:], b_ada, writes=[b_badar])
            cx.dma("sp", n1gc[:], n1g_col, writes=[b_n1gc])
            cx.dma("sp", n2gr[:], n2g_rep, writes=[b_n2gr])
            cx.dma("sp", lv[:], lamv, writes=[b_lv])
            cx.op("act", lambda e: e.activation(out=cact[:], in_=ccol[:], func=AF.Silu), reads=[b_ccol], writes=[b_cact])
            def p0_main():
                wq = ["sp", "act"]
                for g in range(12):
                    wt, b_wt = wbig[g % 3]
                    cx.dma(wq[g % 2], wt[:], kcp(w_ada)[:, :, g * 512:(g + 1) * 512], writes=[b_wt])
                    ps, b_ps = prot.next()
                    for kc in range(8):
                        cx.op("pe", lambda e: e.matmul(ps[0:1, :], lhsT=cact[:, kc:kc + 1], rhs=wt[:, kc, :],
                                                       start=(kc == 0), stop=(kc == 7)),
                              reads=[b_cact, b_wt], writes=[b_ps])
                    cx.op("dve", lambda e: e.tensor_tensor(out=modr[0:1, g * 512:(g + 1) * 512], in0=ps[0:1, :],
                                                           in1=badar[0:1, g * 512:(g + 1) * 512], op=ALU.add),
                          reads=[b_ps, b_badar], writes=[b_modr])
                ps, b_ps = prot.next()
                for jn in range(16):
                    off = (0 if jn < 8 else 1024) + (jn % 8) * 128
                    cx.op("pe", lambda e: e.matmul(ps[:, jn:jn + 1], lhsT=modr[0:1, off:off + 128], rhs=ones_f[0:1, 0:1],
                                                   start=True, stop=True), reads=[b_modr, b_cst], writes=[b_ps])
                cx.op("dve", lambda e: e.tensor_copy(out=sh1c[:], in_=ps[:, 0:8]), reads=[b_ps], writes=[b_sh1c])
                cx.op("dve", lambda e: e.scalar_tensor_tensor(out=s1c[:], in0=ps[:, 8:16], scalar=1.0, in1=n1gc[:],
                                                              op0=ALU.add, op1=ALU.mult),
                      reads=[b_ps, b_n1gc], writes=[b_s1c])
                for (dst, b_dst, off) in ((g1rep, b_g1rep, 2048), (sh2rep, b_sh2rep, 3072),
                                          (a2rep, b_a2rep, 4096), (g2rep, b_g2rep, 5120)):
                    for half in range(2):
                        ps, b_ps = prot.next()
                        cx.op("pe", lambda e: e.matmul(ps[:, :], lhsT=ones_f[0:1, 0:128],
                                                       rhs=modr[0:1, off + half * 512:off + (half + 1) * 512],
                                                       start=True, stop=True), reads=[b_modr, b_cst], writes=[b_ps])
                        cx.op("dve", lambda e: e.tensor_copy(out=dst[:, half * 512:(half + 1) * 512], in_=ps[:, :]),
                              reads=[b_ps], writes=[b_dst])
                cx.op("dve", lambda e: e.scalar_tensor_tensor(out=a2rep[:], in0=a2rep[:], scalar=1.0, in1=n2gr[:],
                                                              op0=ALU.add, op1=ALU.mult),
                      reads=[b_n2gr, b_a2rep], writes=[b_a2rep])
                cx.dma("sp", REPS[0], a2rep[:], reads=[b_a2rep])
                cx.dma("sp", REPS[1], sh2rep[:], reads=[b_sh2rep])
                cx.dma("sp", REPS[2], g2rep[:], reads=[b_g2rep])
                for kc in range(8):
                    cx.op("dve", lambda e: e.tensor_scalar(out=sh1rep[:, kc, :], in0=ones_f[:, 0:33], scalar1=sh1c[:, kc:kc + 1],
                                                           scalar2=None, op0=ALU.mult), reads=[b_cst, b_sh1c], writes=[b_sh1rep])
                cx.op("pool", lambda e: e.memset(bias_hl[:], 0.0), writes=[b_bias_hl])
                psbc, b_psbc = prot.next()
                engs3 = ["act", "dve", "dve"]
                for g in range(7):
                    wt, b_wt = wbig[g % 3]
                    cx.dma(wq[g % 2], wt[:], kcp(w_in)[:, :, g * 512:(g + 1) * 512], writes=[b_wt])
                    ps, b_ps = prot.next()
                    if ps is psbc:
                        ps, b_ps = prot.next()
                    for kc in range(8):
                        cx.op("pe", lambda e: e.matmul(ps[0:33, :], lhsT=sh1rep[:, kc, :], rhs=wt[:, kc, :],
                                                       start=(kc == 0), stop=(kc == 7)),
                              reads=[b_sh1rep, b_wt], writes=[b_ps])
                    cx.op("dve", lambda e: e.tensor_copy(out=biasr[:, g * 512:(g + 1) * 512], in_=ps[0:33, :]),
                          reads=[b_ps], writes=[b_biasr])
                    if g < 2:
                        for sub in range(4):
                            for kc in range(8):
                                cx.op("pe", lambda e: e.matmul(psbc[:, g * 4 + sub:g * 4 + sub + 1],
                                                               lhsT=wt[:, kc, sub * 128:(sub + 1) * 128],
                                                               rhs=sh1c[:, kc:kc + 1], start=(kc == 0), stop=(kc == 7)),
                                      reads=[b_sh1c, b_wt], writes=[b_psbc])
                    wc, b_wc = wcast[g % 3]
                    for kc in range(8):
                        en = engs3[kc % 3]
                        if en == "act":
                            cx.op("act", lambda e: e.activation(out=wc[:, kc, :], in_=wt[:, kc, :], func=AF.Copy,
                                                                scale=s1c[:, kc:kc + 1]),
                                  reads=[b_wt, b_s1c], writes=[b_wc])
                        else:
                            cx.op(en, lambda e: e.tensor_scalar(out=wc[:, kc, :], in0=wt[:, kc, :],
                                                                scalar1=s1c[:, kc:kc + 1], scalar2=None, op0=ALU.mult),
                                  reads=[b_wt, b_s1c], writes=[b_wc])
                    cx.dma("sp", kcp(WP)[:, :, g * 512:(g + 1) * 512], wc[:], reads=[b_wc])
                cx.op("dve", lambda e: e.tensor_copy(out=biascol[:], in_=psbc[:, 0:8]), reads=[b_psbc], writes=[b_biascol])
                cx.op("dve", lambda e: e.tensor_copy(out=bias_hl[0:1, :], in_=biasr[0:1, :]), reads=[b_biasr], writes=[b_bias_hl])
                cx.op("dve", lambda e: e.tensor_copy(out=bh32[32:33, :], in_=biasr[32:33, :]), reads=[b_biasr], writes=[b_bh32])
                cx.op("dve", lambda e: e.tensor_copy(out=bt32[32:33, :], in_=bh32[32:33, :]), reads=[b_bh32], writes=[b_bt32])
                cx.op("dve", lambda e: e.tensor_tensor(out=bt32[32:33, :], in0=biasr[32:33, :], in1=bt32[32:33, :], op=ALU.subtract),
                      reads=[b_biasr, b_bt32], writes=[b_bt32])
                cx.op("dve", lambda e: e.tensor_copy(out=bias_hl[32:33, :], in_=bt32[32:33, :]), reads=[b_bt32, b_bias_hl], writes=[b_bias_hl])
                for half in range(2):
                    wt, b_wt = wbig[(7 + half) % 3]
                    cx.dma(wq[half], wt[:], kcp(w_out)[:, :, half * 512:(half + 1) * 512], writes=[b_wt])
                    wc, b_wc = wcast[(7 + half) % 3]
                    for kc in range(8):
                        en = "dve"
                        cx.op(en, lambda e: e.tensor_tensor(out=wc[:, kc, :], in0=wt[:, kc, :],
                                                            in1=g1rep[:, half * 512:(half + 1) * 512], op=ALU.mult),
                              reads=[b_wt, b_g1rep], writes=[b_wc])
                    cx.dma("sp", kcp(WOP)[:, :, half * 512:(half + 1) * 512], wc[:], reads=[b_wc])

            def p0_misc():
                for i2 in range(2):
                    cx.op("dve", lambda e: e.tensor_tensor(out=ltmp[:, 0:64], in0=lv[:, i2 * 128:i2 * 128 + 64],
                                                           in1=lv[:, i2 * 128 + 64:i2 * 128 + 128], op=ALU.mult),
                          reads=[b_lv], writes=[b_ltmp])
                    cx.op("dve", lambda e: e.tensor_reduce(out=lsum[:, i2:i2 + 1], in_=ltmp[:, 0:64], axis=AX.X, op=ALU.add),
                          reads=[b_ltmp], writes=[b_lsum])
                cx.op("act", lambda e: e.activation(out=lsum[:], in_=lsum[:], func=AF.Exp), reads=[b_lsum], writes=[b_lsum])
                cx.op("dve", lambda e: e.scalar_tensor_tensor(out=neglam[:], in0=lsum[:, 1:2], scalar=-LAM_INIT,
                                                              in1=lsum[:, 0:1], op0=ALU.add, op1=ALU.subtract),
                      reads=[b_lsum], writes=[b_neglam])
                cx.op("dve", lambda e: e.tensor_scalar(out=sgr[:], in0=sgr[:], scalar1=1.0 - LAM_INIT, scalar2=None,
                                                       op0=ALU.mult), reads=[b_sgr], writes=[b_sgr])
                cx.op("dve", lambda e: e.tensor_scalar(out=negb31[:], in0=tbr[:, 124:128], scalar1=-1.0, scalar2=None,
                                                       op0=ALU.mult), reads=[b_tbr], writes=[b_negb31])
                for D in range(2):
                    for h in range(4):
                        cx.op("pool", lambda e: e.memset(bacc[h][0][:], 0.0), writes=[bacc[h][1]])
                    for b in range(32):
                        cx.op("dve", lambda e: e.tensor_scalar(out=eq[:], in0=idxD[D], scalar1=float(b), scalar2=None,
                                                               op0=ALU.is_equal), reads=[b_cst], writes=[b_eq])
                        for h in range(4):
                            en = "dve"
                            cx.op(en, lambda e: e.scalar_tensor_tensor(out=bacc[h][0][:], in0=eq[:],
                                                                       scalar=tbr[:, b * 4 + h:b * 4 + h + 1],
                                                                       in1=bacc[h][0][:], op0=ALU.mult, op1=ALU.add),
                                  reads=[b_eq, b_tbr, bacc[h][1]], writes=[bacc[h][1]])
                    for h in range(4):
                        cx.op("act", lambda e: e.activation(out=bacc[h][0][:], in_=bacc[h][0][:], func=AF.Exp,
                                                            bias=negb31[:, h:h + 1]),
                              reads=[bacc[h][1], b_negb31], writes=[bacc[h][1]])
                        if D == 0:
                            cx.op("dve", lambda e: e.tensor_tensor(out=EB[:, h * 2 + D, :], in0=bacc[h][0][:], in1=MT,
                                                                   op=ALU.mult), reads=[bacc[h][1], b_cst], writes=[b_EB])
                        else:
                            cx.op("dve", lambda e: e.tensor_copy(out=EB[:, h * 2 + D, :], in_=bacc[h][0][:]),
                                  reads=[bacc[h][1]], writes=[b_EB])

            cx.zip_emit([p0_main, p0_misc], weights=[1, 1])
            cx.barrier()

        with ExitStack() as stkv:
            KT = stkv.enter_context(nc.sbuf_tensor("KT", [128, 4, S], BF16))
            b_KT = [Buf() for _ in range(NJ)]
            VA = stkv.enter_context(nc.sbuf_tensor("VA", [128, NT, 4, 129], BF16))
            b_VA = [Buf() for _ in range(NT)]
            for tt in range(NT):
                cx.op("pool", lambda e: e.memset(VA[:, tt, :, 128:129], 1.0), writes=[b_VA[tt]])

            with ExitStack() as st:
                psf = [PB(st, "p1a_%d" % i, [128, 512], F32) for i in range(6)]
                prot = Rot(psf)
                pst = [PB(st, "p1aT_%d" % i, [128, 8, 128], BF16) for i in range(2)]
                xin = [TB(st, "xin%d" % i, [128, 1024], F32) for i in range(3)]
                ssq = [TB(st, "ssq%d" % i, [128, 4], F32) for i in range(4)]
                xh = [TB(st, "xh%d" % i, [128, 1024], BF16) for i in range(5)]
                xT = [st.enter_context(nc.sbuf_tensor("xT%d" % i, [128, 8, 512], BF16)) for i in range(2)]
                b_xT = [[Buf() for _ in range(4)] for _ in range(2)]
                wp = [TB(st, "wp%d" % i, [128, 8, 512], BF16) for i in range(2)]
                stg_b = [TB(st, "stgb%d" % i, [128, 512], BF16) for i in range(3)]
                stg_f = [TB(st, "stgf%d" % i, [128, 512], F32) for i in range(2)]
                sb_rot = Rot(stg_b)
                sf_rot = Rot(stg_f)
                ecount = [0]

                def ldx(tt):
                    xi, b_xi = xin[tt % 3]
                    cx.dma("sp", xi[:], x[tt * 128:(tt + 1) * 128, :], writes=[b_xi])

                def norm(tt):
                    xi, b_xi = xin[tt % 3]
                    sq, b_sq = ssq[tt % 4]
                    xhh, b_xhh = xh[tt % 5]
                    cx.op("act", lambda e: e.activation(out=xhh[:], in_=xi[:], func=AF.Square,
                                                        accum_out=sq[:, 0:1]), reads=[b_xi], writes=[b_xhh, b_sq])
                    rstd_from_ssq(sq[:, 0:1], sq[:, 1:2], sq[:, 2:3], 1024.0, [b_sq], [b_sq])
                    cx.op("dve", lambda e: e.tensor_scalar(out=xhh[:], in0=xi[:], scalar1=sq[:, 1:2], scalar2=None,
                                                           op0=ALU.mult), reads=[b_xi, b_sq], writes=[b_xhh])
                    if tt + 3 < NT:
                        ldx(tt + 3)

                def transp(tt):
                    j, t = divmod(tt, 4)
                    xhh, b_xhh = xh[tt % 5]
                    pT, b_pT = pst[tt % 2]
                    xTj = xT[j % 2]
                    bx = b_xT[j % 2]
                    for kc in range(8):
                        cx.op("pe", lambda e: e.transpose(out=pT[:, kc, :], in_=xhh[:, kc * 128:(kc + 1) * 128],
                                                          identity=ident_b), reads=[b_xhh, b_cbf], writes=[b_pT])
                    cx.op("act", lambda e: e.activation(out=xTj[:, 0:4, t * 128:(t + 1) * 128], in_=pT[:, 0:4, :],
                                                        func=AF.Copy), reads=[b_pT], writes=[bx[t]])
                    cx.op("dve", lambda e: e.tensor_copy(out=xTj[:, 4:8, t * 128:(t + 1) * 128], in_=pT[:, 4:8, :]),
                          reads=[b_pT], writes=[bx[t]])

                def ldw(j, g):
                    w, b_w = wp[(j * 7 + g) % 2]
                    cx.dma("act" if g % 2 else "sp", w[:], kcp(WP)[:, :, g * 512:(g + 1) * 512], writes=[b_w])

                def group(j, g):
                    w, b_w = wp[(j * 7 + g) % 2]
                    xTj = xT[j % 2]
                    bx = b_xT[j % 2]
                    if g < 2:
                        for h in range(4):
                            ps, b_ps = prot.next()
                            for kc in range(8):
                                cx.op("pe", lambda e: e.matmul(ps[:, :], lhsT=w[:, kc, h * 128:(h + 1) * 128],
                                                               rhs=xTj[:, kc, :], start=(kc == 0), stop=(kc == 7)),
                                      reads=[b_w] + bx, writes=[b_ps])
                            if g == 0:
                                sg_, b_sg = sb_rot.next()
                                cx.op("act", lambda e: e.activation(out=sg_[:], in_=ps[:, :], func=AF.Identity,
                                                                    bias=biascol[:, h:h + 1]),
                                      reads=[b_ps, b_biascol], writes=[b_sg])
                                cx.dma("sp", QT[h, :, j * 512:(j + 1) * 512], sg_[:], reads=[b_sg])
                            else:
                                cx.op("act", lambda e: e.activation(out=KT[:, h, j * 512:(j + 1) * 512], in_=ps[:, :],
                                                                    func=AF.Identity, bias=biascol[:, 4 + h:5 + h]),
                                      reads=[b_ps, b_biascol], writes=[b_KT[j]])
                    else:
                        for t in range(4):
                            tt = 4 * j + t
                            ps, b_ps = prot.next()
                            for kc in range(8):
                                cx.op("pe", lambda e: e.matmul(ps[:, :], lhsT=xTj[:, kc, t * 128:(t + 1) * 128],
                                                               rhs=w[:, kc, :], start=(kc == 0), stop=False),
                                      reads=[b_w, bx[t]], writes=[b_ps])
                            cx.op("pe", lambda e: e.matmul(ps[:, :], lhsT=ones_b[0:33, 0:128],
                                                           rhs=bias_hl[0:33, g * 512:(g + 1) * 512], start=False, stop=True),
                                  reads=[b_cbf, b_bias_hl], writes=[b_ps])
                            en = "act" if g in (3, 4, 6) else "dve"
                            afn = {3: AF.Silu, 4: AF.Sigmoid, 6: AF.Silu}.get(g, AF.Copy)
                            if g == 2:
                                dst = VA[:, tt, :, 0:128]
                                src = ps[:, :].rearrange("p (h v) -> p h v", h=4)
                                wb = b_VA[tt]
                            elif g == 4:
                                sg_, wb = sf_rot.next()
                                dst = sg_[:]
                                src = ps[:, :]
                            else:
                                sg_, wb = sb_rot.next()
                                dst = sg_[:]
                                src = ps[:, :]
                            if en == "act":
                                cx.op("act", lambda e: e.activation(out=dst, in_=src, func=afn),
                                      reads=[b_ps], writes=[wb])
                            else:
                                cx.op("dve", lambda e: e.tensor_copy(out=dst, in_=src), reads=[b_ps], writes=[wb])
                            if g != 2:
                                dt_ = {3: QR, 4: FR, 5: IR, 6: GR}[g]
                                cx.dma("sp", dt_[tt * 128:(tt + 1) * 128, :], dst, reads=[wb])

                for tt in range(min(3, NT)):
                    ldx(tt)
                ldw(0, 0)
                for tt in range(4):
                    norm(tt)
                    transp(tt)
                for j in range(NJ):
                    for g in range(7):
                        if g < 6:
                            ldw(j, g + 1)
                        elif j + 1 < NJ:
                            ldw(j + 1, 0)
                        if j + 1 < NJ and g < 4:
                            norm(4 * (j + 1) + g)
                        group(j, g)
                        if j + 1 < NJ and 2 <= g < 6:
                            transp(4 * (j + 1) + g - 2)
                cx.barrier()

            with ExitStack() as st:
                pss = [PB(st, "p1bs_%d" % a, [128, 2, 512], F32) for a in range(2)]
                acc = [PB(st, "p1ba_%d" % a, [128, 512], F32) for a in range(3)]
                accsb = [TB(st, "accsb%d" % a, [128, 3, 387], F32) for a in range(2)]
                qt = [TB(st, "qt%d" % i, [128, 512], BF16) for i in range(2)]
                pT = [TB(st, "pT%d" % a, [128, 2, 512], BF16) for a in range(2)]
                yast = [TB(st, "yast%d" % i, [128, 4, 512], BF16) for i in range(2)]
                sm = [TB(st, "sm%d" % i, [128, 24], F32) for i in range(4)]
                evT = [TB(st, "evT%d" % i, [128, 8, 128], F32) for i in range(2)]
                evO = [TB(st, "evO%d" % i, [128, 4, 128], F32) for i in range(2)]
                evQ = [TB(st, "evQ%d" % i, [128, 4, 128], F32) for i in range(2)]
                smrot = Rot(sm)
                tmpA = [TB(st, "tmpA%d" % i, [128, 128], F32) for i in range(2)]
                ob = [TB(st, "ob%d" % i, [128, 128], F32) for i in range(2)]
                jk2, b_jk2 = TB(st, "jk2", [128, 128], BF16)
                scale = 64 ** -0.5
                pT3 = [TB(st, "pT3_%d" % a, [128, 2, 512], BF16) for a in range(3)]
                heads = [(j, h) for j in range(NJ) for h in range(4)]
                steps = []
                for m, (j, h) in enumerate(heads):
                    for i in range(4 * j + 4):
                        steps.append((m, j, h, i))
                N = len(steps)
                started = {}

                def load_q(m):
                    j, h = heads[m]
                    q, b_q = qt[m % 2]
                    cx.dma("sp", q[:], QT[h, :, j * 512:(j + 1) * 512], writes=[b_q])

                def emit_qk(n):
                    m, j, h, i = steps[n]
                    q, b_q = qt[m % 2]
                    c0 = max(0, i - 4 * j) * 128
                    ps, b_ps = pss[n % 2]
                    for c in range(2):
                        cx.op("pe", lambda e: e.matmul(ps[:, c, c0:512], lhsT=KT[c * 64:(c + 1) * 64, h, i * 128:(i + 1) * 128],
                                                       rhs=q[c * 64:(c + 1) * 64, c0:512], start=True, stop=True),
                              reads=[b_KT[i // 4], b_q], writes=[b_ps])

                def emit_exp(n):
                    m, j, h, i = steps[n]
                    c0 = max(0, i - 4 * j) * 128
                    ps, b_ps = pss[n % 2]
                    p_, b_p = pT3[n % 3]
                    cx.op("act", lambda e: e.activation(out=p_[:, :, c0:512], in_=ps[:, :, c0:512], func=AF.Exp, scale=scale),
                          reads=[b_ps], writes=[b_p])
                    for s in range(4):
                        D = 4 * j + s - i
                        if D in (0, 1):
                            cx.op("dve", lambda e: e.tensor_tensor(out=p_[:, :, s * 128:(s + 1) * 128],
                                                                   in0=p_[:, :, s * 128:(s + 1) * 128],
                                                                   in1=EB[:, h * 2 + D, :].unsqueeze(1).to_broadcast([128, 2, 128]),
                                                                   op=ALU.mult),
                                  reads=[b_EB, b_p], writes=[b_p])

                def emit_pv(n):
                    m, j, h, i = steps[n]
                    p_, b_p = pT3[n % 3]
                    for s in range(max(0, i - 4 * j), 4):
                        for c in range(2):
                            ri = s * 2 + c
                            a_, b_a = acc[ri // 3]
                            r0 = (ri % 3) * 129
                            first = (m, ri // 3) not in started
                            started[(m, ri // 3)] = True
                            cx.op("pe", lambda e: e.matmul(a_[:, r0:r0 + 129], lhsT=p_[:, c, s * 128:(s + 1) * 128],
                                                           rhs=VA[:, i, h, :], start=first, stop=(i == 4 * j + s),
                                                           skip_group_check=True),
                                  reads=[b_p, b_VA[i]], writes=[b_a])
                    if i == 4 * j + 3:
                        emit_evac(m)

                def emit_evac(m):
                    j, h = heads[m]
                    ya, b_ya = yast[j % 2]
                    asb, b_asb = accsb[m % 2]
                    for bk in range(3):
                        nreg = 3 if bk < 2 else 2
                        cx.op("dve", lambda e: e.tensor_copy(out=asb[:, bk, 0:nreg * 129], in_=acc[bk][0][:, 0:nreg * 129]),
                              reads=[acc[bk][1]], writes=[b_asb])
                    flat = asb[:, :, :].rearrange("p a b -> p (a b)")
                    reg = flat[:, 0:8 * 129].rearrange("p (r c) -> p r c", c=129)
                    m_, b_m = smrot.next()
                    T_, b_T = evT[m % 2]
                    o4, b_o4 = evO[m % 2]
                    q4, b_q4 = evQ[m % 2]
                    b_a = b_asb
                    cx.op("dve", lambda e: e.reciprocal(out=m_[:, 0:8], in_=flat[:, 128:128 + 8 * 129:129]), reads=[b_a], writes=[b_m])
                    cx.op("dve", lambda e: e.tensor_tensor(out=T_[:, :, :], in0=reg[:, :, 0:128],
                                                           in1=m_[:, 0:8].unsqueeze(2).to_broadcast([128, 8, 128]), op=ALU.mult),
                          reads=[b_a, b_m], writes=[b_T])
                    Tv = T_[:, :, :].rearrange("p (s c) v -> p s c v", c=2)
                    cx.op("dve", lambda e: e.scalar_tensor_tensor(out=o4[:, :, :], in0=Tv[:, :, 1, :], scalar=neglam[:, 0:1],
                                                                  in1=Tv[:, :, 0, :], op0=ALU.mult, op1=ALU.add),
                          reads=[b_T, b_neglam], writes=[b_o4])
                    cx.op("dve", lambda e: e.tensor_tensor(out=q4[:, :, :], in0=o4[:, :, :], in1=o4[:, :, :], op=ALU.mult),
                          reads=[b_o4], writes=[b_q4])
                    cx.op("dve", lambda e: e.tensor_reduce(out=m_[:, 8:12], in_=q4[:, :, :], axis=AX.X, op=ALU.add),
                          reads=[b_q4, b_m], writes=[b_m])
                    rstd_from_ssq(m_[:, 8:12], m_[:, 12:16], m_[:, 16:20], 128.0, [b_m], [b_m])
                    cx.op("dve", lambda e: e.tensor_tensor(out=q4[:, :, :], in0=o4[:, :, :],
                                                           in1=m_[:, 12:16].unsqueeze(2).to_broadcast([128, 4, 128]), op=ALU.mult),
                          reads=[b_o4, b_m, b_q4], writes=[b_q4])
                    cx.op("dve", lambda e: e.tensor_tensor(out=ya[:, :, h * 128:(h + 1) * 128], in0=q4[:, :, :],
                                                           in1=sgr[:, :].unsqueeze(1).to_broadcast([128, 4, 128]), op=ALU.mult),
                          reads=[b_q4, b_sgr], writes=[b_ya])
                    if h == 3:
                        cx.dma("sp", MIX[j * 512:(j + 1) * 512, 0:512].rearrange("(s p) c -> p s c", p=128), ya[:], reads=[b_ya])

                conv = [(wi, e) for e in range(32) for wi in range(3)]
                conv.reverse()

                def emit_conv(k):
                    for _ in range(k):
                        if conv:
                            wi, e = conv.pop()
                            src = (w1r, w3r, w2r)[wi]
                            cx.dma("pool", WB[wi][e * 128:(e + 1) * 128, :], src[e * 128:(e + 1) * 128, :])

                load_q(0)
                emit_qk(0)
                for n in range(N):
                    m, j, h, i = steps[n]
                    if i == 0:
                        emit_conv(2)
                    if i == 0 and m + 1 < len(heads):
                        load_q(m + 1)
                    emit_exp(n)
                    if n + 1 < N:
                        emit_qk(n + 1)
                    if n >= 1:
                        emit_pv(n - 1)
                emit_pv(N - 1)
                emit_conv(len(conv))
                cx.barrier()

        with ExitStack() as st:
            psA, b_psA = PB(st, "p1c_A", [128, 512], F32)
            psB, b_psB = PB(st, "p1c_B", [128, 512], F32)
            psC, b_psC = PB(st, "p1c_C", [128, 512], F32)
            psS, b_psS = PB(st, "p1c_S", [128, 512], F32)
            psO, b_psO = PB(st, "p1c_O", [128, 512], F32)
            psU = [PB(st, "p1c_U%d" % i, [128, 512], F32) for i in range(2)]
            psT, b_psT = PB(st, "p1c_T", [128, 8, 128], BF16)
            ld = [[TB(st, "ld%d_%d" % (a, k), [128, 512], F32 if k == 1 else BF16) for k in range(4)] for a in range(5)]
            sig, b_sig = TB(st, "sig", [128, 512], F32)
            lf_2 = [TB(st, "lf%d" % i, [128, 512], F32) for i in range(2)]
            kf_2 = [TB(st, "kf%d" % i, [128, 512], F32) for i in range(2)]
            e1, b_e1 = TB(st, "e1", [128, 512], F32)
            e1n, b_e1n = TB(st, "e1n", [128, 512], F32)
            e2, b_e2 = TB(st, "e2", [128, 512], F32)
            qs, b_qs = TB(st, "qs", [128, 512], F32)
            gg_2 = [TB(st, "gg%d" % i, [128, 512], F32) for i in range(4)]
            ebl_2 = [TB(st, "ebl%d" % i, [128, 8], F32) for i in range(3)]
            Qd_2 = [TB(st, "Qd%d" % i, [128, 512], BF16) for i in range(2)]
            Kd_2 = [TB(st, "Kd%d" % i, [128, 512], BF16) for i in range(2)]
            Kd2_2 = [TB(st, "Kd2%d" % i, [128, 512], BF16) for i in range(3)]
            QKT_2 = [TB(st, "QKT%d" % i, [128, 8, 128], BF16) for i in range(2)]
            scm_2 = [TB(st, "scm%d" % i, [128, 4, 128], BF16) for i in range(2)]
            Sf = [TB(st, "Sf%d" % h, [128, 128], F32) for h in range(4)]
            Sb = [TB(st, "Sb%d" % h, [128, 128], BF16) for h in range(4)]
            hs_, b_hs = TB(st, "hsq", [128, 16], F32)
            jk3, b_jk3 = TB(st, "jk3", [128, 128], BF16)
            yst = [TB(st, "yst%d" % i, [128, 512], BF16) for i in range(2)]
            yt_, b_yt = TB(st, "ytmp", [128, 4, 128], F32)
            lbr, b_lbr = TB(st, "lbr", [128, 512], F32)
            omlr, b_omlr = TB(st, "omlr", [128, 512], F32)
            rngr, b_rngr = TB(st, "rngr", [128, 512], F32)
            cx.dma("sp", rngr[:], rng_rep, writes=[b_rngr])
            cx.dma("sp", e1[:], rlb_rep[:, 0:512], writes=[b_e1])
            cx.dma("sp", e2[:], rlb_rep[:, 512:1024], writes=[b_e2])
            cx.op("dve", lambda e: e.tensor_tensor(out=lbr[:], in0=e1[:], in1=e2[:], op=ALU.subtract),
                  reads=[b_e1, b_e2], writes=[b_lbr])
            cx.op("act", lambda e: e.activation(out=lbr[:], in_=lbr[:], func=AF.Sigmoid), reads=[b_lbr], writes=[b_lbr])
            cx.op("dve", lambda e: e.tensor_scalar(out=omlr[:], in0=lbr[:], scalar1=-1.0, scalar2=1.0,
                                                   op0=ALU.mult, op1=ALU.add), reads=[b_lbr], writes=[b_omlr])
            for h in range(4):
                cx.op("pool", lambda e: e.memset(Sf[h][0][:], 0.0), writes=[Sf[h][1]])
                cx.op("pool", lambda e: e.memset(Sb[h][0][:], 0.0), writes=[Sb[h][1]])
            def ld1c(tt):
                (qr_, b_qr), (fr_, b_fr), (ir_, b_ir), (gr_, b_gr) = ld[tt % 5]
                rows = slice(tt * 128, (tt + 1) * 128)
                cx.dma("sp", qr_[:], QR[rows, :], writes=[b_qr])
                cx.dma("sp", fr_[:], FR[rows, :], writes=[b_fr])
                cx.dma("sp", ir_[:], IR[rows, :], writes=[b_ir])
                cx.dma("sp", gr_[:], GR[rows, :], writes=[b_gr])

            def stA0_1c(tt):
                (qr_, b_qr), (fr_, b_fr), (ir_, b_ir), (gr_, b_gr) = ld[tt % 5]
                gg, b_gg = gg_2[tt % 4]
                lf, b_lf = lf_2[tt % 2]
                kf, b_kf = kf_2[tt % 2]
                if tt + 1 < NT:
                    ld1c(tt + 1)
                cx.op("dve", lambda e: e.tensor_tensor(out=sig[:], in0=fr_[:], in1=omlr[:], op=ALU.mult),
                      reads=[b_fr, b_omlr], writes=[b_sig])
                cx.op("dve", lambda e: e.tensor_tensor(out=sig[:], in0=sig[:], in1=lbr[:], op=ALU.add),
                      reads=[b_sig, b_lbr], writes=[b_sig])
                cx.op("act", lambda e: e.activation(out=lf[:], in_=sig[:], func=AF.Ln), reads=[b_sig], writes=[b_lf])
                cx.op("act", lambda e: e.activation(out=kf[:], in_=sig[:], func=AF.Identity, scale=-1.0, bias=1.0),
                      reads=[b_sig], writes=[b_kf])
                cx.op("dve", lambda e: e.tensor_tensor(out=gg[:], in0=gr_[:], in1=rngr[:], op=ALU.mult),
                      reads=[b_gr, b_rngr], writes=[b_gg])

            def stA1c(tt):
                (qr_, b_qr), (fr_, b_fr), (ir_, b_ir), (gr_, b_gr) = ld[tt % 5]
                lf, b_lf = lf_2[tt % 2]
                kf, b_kf = kf_2[tt % 2]
                ebl, b_ebl = ebl_2[tt % 3]
                Kd2, b_Kd2 = Kd2_2[tt % 3]
                Qd, b_Qd = Qd_2[tt % 2]
                Kd, b_Kd = Kd_2[tt % 2]
                cx.op("pe", lambda e: e.matmul(psA[:, :], lhsT=L1, rhs=lf[:], start=True, stop=True),
                      reads=[b_cst, b_lf], writes=[b_psA])
                cx.op("pe", lambda e: e.matmul(psB[:, :], lhsT=L2, rhs=lf[:], start=True, stop=True),
                      reads=[b_cst, b_lf], writes=[b_psB])
                for h in range(4):
                    cx.op("pe", lambda e: e.matmul(psC[:, 2 * h:2 * h + 2], lhsT=lf[:, h * 128:(h + 1) * 128], rhs=sel2,
                                                   start=True, stop=True), reads=[b_cst, b_lf], writes=[b_psC])
                cx.op("act", lambda e: e.activation(out=e1[:], in_=psA[:, :], func=AF.Exp), reads=[b_psA], writes=[b_e1])
                cx.op("act", lambda e: e.activation(out=e1n[:], in_=psA[:, :], func=AF.Exp, scale=-1.0),
                      reads=[b_psA], writes=[b_e1n])
                cx.op("act", lambda e: e.activation(out=e2[:], in_=psB[:, :], func=AF.Exp), reads=[b_psB], writes=[b_e2])
                cx.op("act", lambda e: e.activation(out=ebl[:], in_=psC[:, 0:8], func=AF.Exp), reads=[b_psC], writes=[b_ebl])
                cx.op("dve", lambda e: e.tensor_tensor(out=Qd[:], in0=qr_[:], in1=e1[:], op=ALU.mult),
                      reads=[b_qr, b_e1], writes=[b_Qd])
                cx.op("dve", lambda e: e.tensor_tensor(out=Kd[:], in0=kf[:], in1=e1n[:], op=ALU.mult),
                      reads=[b_kf, b_e1n], writes=[b_Kd])
                cx.op("dve", lambda e: e.tensor_tensor(out=Kd2[:], in0=kf[:], in1=e2[:], op=ALU.mult),
                      reads=[b_kf, b_e2], writes=[b_Kd2])

            def stA2_1c(tt):
                Qd, b_Qd = Qd_2[tt % 2]
                Kd, b_Kd = Kd_2[tt % 2]
                QKT, b_QKT = QKT_2[tt % 2]
                scm, b_scm = scm_2[tt % 2]
                for h in range(4):
                    cx.op("pe", lambda e: e.transpose(out=psT[:, h, :], in_=Qd[:, h * 128:(h + 1) * 128], identity=ident_b),
                          reads=[b_Qd, b_cbf], writes=[b_psT])
                for h in range(4):
                    cx.op("pe", lambda e: e.transpose(out=psT[:, 4 + h, :], in_=Kd[:, h * 128:(h + 1) * 128], identity=ident_b),
                          reads=[b_Kd, b_cbf], writes=[b_psT])
                cx.op("act", lambda e: e.activation(out=QKT[:, 0:4, :], in_=psT[:, 0:4, :], func=AF.Copy),
                      reads=[b_psT], writes=[b_QKT])
                cx.op("dve", lambda e: e.tensor_copy(out=QKT[:, 4:8, :], in_=psT[:, 4:8, :]), reads=[b_psT], writes=[b_QKT])
                for h in range(4):
                    cx.op("pe", lambda e: e.matmul(psS[:, h * 128:(h + 1) * 128], lhsT=QKT[:, 4 + h, :], rhs=QKT[:, h, :],
                                                   start=True, stop=True), reads=[b_QKT], writes=[b_psS])
                cx.op("dve", lambda e: e.tensor_tensor(out=scm[:, :, :], in0=psS[:, :].rearrange("p (h t) -> p h t", h=4),
                                                       in1=MT.unsqueeze(1).to_broadcast([128, 4, 128]), op=ALU.mult),
                      reads=[b_psS, b_cst], writes=[b_scm])

            def stB1c(tt):
                (qr_, b_qr), (fr_, b_fr), (ir_, b_ir), (gr_, b_gr) = ld[tt % 5]
                gg, b_gg = gg_2[tt % 4]
                ebl, b_ebl = ebl_2[tt % 3]
                Kd2, b_Kd2 = Kd2_2[tt % 3]
                QKT, b_QKT = QKT_2[tt % 2]
                scm, b_scm = scm_2[tt % 2]
                rows = slice(tt * 128, (tt + 1) * 128)
                for h in range(4):
                    hv = slice(h * 128, (h + 1) * 128)
                    cx.op("act", lambda e: e.activation(out=Sb[h][0][:], in_=Sf[h][0][:], func=AF.Copy, scale=ebl[:, 2 * h + 1:2 * h + 2]),
                          reads=[Sf[h][1], b_ebl], writes=[Sb[h][1]])
                    cx.op("pe", lambda e: e.matmul(psO[:, hv], lhsT=QKT[:, h, :], rhs=Sb[h][0][:], start=True, stop=False),
                          reads=[b_QKT, Sb[h][1]], writes=[b_psO])
                    cx.op("pe", lambda e: e.matmul(psO[:, hv], lhsT=scm[:, h, :], rhs=ir_[:, hv], start=False, stop=True),
                          reads=[b_scm, b_ir], writes=[b_psO])
                    pu, b_pu = psU[h % 2]
                    cx.op("pe", lambda e: e.matmul(pu[:, 0:128], lhsT=Kd2[:, hv], rhs=ir_[:, hv], start=True, stop=True),
                          reads=[b_Kd2, b_ir], writes=[b_pu])
                    cx.op("dve", lambda e: e.scalar_tensor_tensor(out=Sf[h][0][:], in0=Sf[h][0][:], scalar=ebl[:, 2 * h:2 * h + 1],
                                                                  in1=pu[:, 0:128], op0=ALU.mult, op1=ALU.add),
                          reads=[b_ebl, b_pu, Sf[h][1]], writes=[Sf[h][1]])
                for h in range(4):
                    cx.op("act", lambda e: e.activation(out=jk3[:], in_=psO[:, h * 128:(h + 1) * 128], func=AF.Square,
                                                        accum_out=hs_[:, h:h + 1]), reads=[b_psO], writes=[b_jk3, b_hs])
                rstd_from_ssq(hs_[:, 0:4], hs_[:, 4:8], hs_[:, 8:12], 128.0, [b_hs], [b_hs], via="act")
                y_, b_y = yst[tt % 2]
                cx.op("dve", lambda e: e.tensor_tensor(out=yt_[:, :, :], in0=psO[:, :].rearrange("p (h v) -> p h v", h=4),
                                                       in1=hs_[:, 4:8].unsqueeze(2).to_broadcast([128, 4, 128]), op=ALU.mult),
                      reads=[b_psO, b_hs], writes=[b_yt])
                cx.op("dve", lambda e: e.tensor_tensor(out=y_[:, :], in0=yt_[:, :, :].rearrange("p h v -> p (h v)"), in1=gg[:, :],
                                                       op=ALU.mult), reads=[b_yt, b_gg], writes=[b_y])
                cx.dma("sp", MIX[rows, 512:1024], y_[:], reads=[b_y])

            ld1c(0)
            for it in range(-3, NT):
                fns = []
                if 0 <= it + 3 < NT:
                    fns.append(lambda: stA0_1c(it + 3))
                if 0 <= it + 2 < NT:
                    fns.append(lambda: stA1c(it + 2))
                if 0 <= it + 1 < NT:
                    fns.append(lambda: stA2_1c(it + 1))
                if it >= 0:
                    fns.append(lambda: stB1c(it))
                cx.zip_emit(fns)
            cx.barrier()

        with ExitStack() as st:
            psX = [PB(st, "p1d_X%d" % i, [128, 512], F32) for i in range(2)]
            psT, b_psT = PB(st, "p1d_T", [128, 8, 128], BF16)
            psH = [PB(st, "p1d_H%d" % i, [128, 512], F32) for i in range(2)]
            psL, b_psL = PB(st, "p1d_L", [128, 512], F32)
            psR, b_psR = PB(st, "p1d_R", [128, 512], F32)
            wop, b_wop = TB(st, "wop", [128, 8, 1024], BF16)
            wrt, b_wrt = TB(st, "wrt", [128, 8, 36], F32)
            brr, b_brr = TB(st, "brr", [128, 36], F32)
            mix = [TB(st, "mix%d" % i, [128, 1024], BF16) for i in range(3)]
            mixT, b_mixT = TB(st, "mixT", [128, 8, 128], BF16)
            xin = [TB(st, "xin1d%d" % i, [128, 1024], F32) for i in range(3)]
            x1 = [TB(st, "x1_%d" % i, [128, 1024], F32) for i in range(2)]
            h2_2 = [TB(st, "h2_%d" % i, [128, 1024], F32) for i in range(2)]
            h2b = [TB(st, "h2b%d" % i, [128, 1024], BF16) for i in range(2)]
            h2T, b_h2T = TB(st, "h2T", [128, 8, 128], F32)
            jk4, b_jk4 = TB(st, "jk4", [128, 1024], BF16)
            sq, b_sq = TB(st, "sq1d", [128, 4], F32)
            lg_2 = [TB(st, "lg%d" % i, [128, 36], F32) for i in range(2)]
            lgT, b_lgT = TB(st, "lgT", [36, 128], F32)
            rt, b_rt = TB(st, "rt", [128, 16], F32)
            gsel, b_gsel = TB(st, "gsel", [128, 4], F32)
            ge, b_ge = TB(st, "ge", [128, 4], F32)
            emask, b_emask = TB(st, "emask", [128, 32], F32)
            elm, b_elm = TB(st, "elm", [128, 32], F32)
            t32_, b_t32 = TB(st, "t32r", [128, 32], F32)
            OH, _ = TB(st, "OH", [128, NT * 2, 32], F32)
            b_OHt = [Buf() for _ in range(NT)]
            cntb2 = [TB(st, "cntb%d" % i, [128, 32], BF16) for i in range(2)]
            cnt2 = [TB(st, "cnt32_%d" % i, [128, 32], F32) for i in range(2)]
            elmb, b_elmb = TB(st, "elmb", [128, 32], F32)
            cnts, b_cnts = TB(st, "cnts", [128, 32], F32)
            cntsb, b_cntsb = TB(st, "cntsb", [128, 32], BF16)
            RK, b_RK = TB(st, "RK", [128, NT * 2], F32)
            tot, b_tot = TB(st, "tot", [128, 32], F32)
            pad, b_pad = TB(st, "pad", [128, 32], F32)
            pend, b_pend = TB(st, "pend", [128, 32], F32)
            pstart, b_pstart = TB(st, "pstart", [128, 32], F32)
            dacc, b_dacc = TB(st, "dacc", [128, NT * 2], F32)
            be, b_be = TB(st, "be", [128, NB], F32)
            a2rep, b_a2rep = TB(st, "a2rep1d", [128, 1024], F32)
            sh2rep, b_sh2rep = TB(st, "sh2rep1d", [128, 1024], F32)
            cx.dma("sp", a2rep[:], REPS[0], writes=[b_a2rep])
            cx.dma("sp", sh2rep[:], REPS[1], writes=[b_sh2rep])
            cx.dma("sp", wop[:], kcp(WOP), writes=[b_wop])
            cx.dma("sp", wrt[:], wr, writes=[b_wrt])
            cx.dma("sp", brr[:], br_rep, writes=[b_brr])
            cx.op("pool", lambda e: e.memset(cnts[:], 0.0), writes=[b_cnts])
            cx.op("pool", lambda e: e.memset(cntsb[:], 0.0), writes=[b_cntsb])
            def ld1d(tt):
                rows = slice(tt * 128, (tt + 1) * 128)
                mx, b_mx = mix[tt % 3]
                xi, b_xi = xin[tt % 3]
                cx.dma("sp", mx[:], MIX[rows, :], writes=[b_mx])
                cx.dma("sp", xi[:], x[rows, :], writes=[b_xi])

            def stC1d(tt):
                rows = slice(tt * 128, (tt + 1) * 128)
                mx, b_mx = mix[tt % 3]
                xi, b_xi = xin[tt % 3]
                h2, b_h2 = h2_2[tt % 2]
                if tt + 1 < NT:
                    ld1d(tt + 1)
                x1_, b_x1 = x1[tt % 2]
                hb, b_hb = h2b[tt % 2]
                for kc in range(8):
                    cx.op("pe", lambda e: e.transpose(out=psT[:, kc, :], in_=mx[:, kc * 128:(kc + 1) * 128], identity=ident_b),
                          reads=[b_mx, b_cbf], writes=[b_psT])
                cx.op("act", lambda e: e.activation(out=mixT[:, 0:4, :], in_=psT[:, 0:4, :], func=AF.Copy),
                      reads=[b_psT], writes=[b_mixT])
                cx.op("dve", lambda e: e.tensor_copy(out=mixT[:, 4:8, :], in_=psT[:, 4:8, :]), reads=[b_psT], writes=[b_mixT])
                for half in range(2):
                    px, b_px = psX[half]
                    for kc in range(8):
                        cx.op("pe", lambda e: e.matmul(px[:, :], lhsT=mixT[:, kc, :], rhs=wop[:, kc, half * 512:(half + 1) * 512],
                                                       start=(kc == 0), stop=(kc == 7)), reads=[b_mixT, b_wop], writes=[b_px])
                    cx.op("dve", lambda e: e.tensor_tensor(out=x1_[:, half * 512:(half + 1) * 512], in0=px[:, :],
                                                           in1=xi[:, half * 512:(half + 1) * 512], op=ALU.add),
                          reads=[b_px, b_xi], writes=[b_x1])
                cx.dma("sp", X1[rows, :], x1_[:], reads=[b_x1])
                cx.op("act", lambda e: e.activation(out=jk4[:], in_=x1_[:], func=AF.Square, accum_out=sq[:, 0:1]),
                      reads=[b_x1], writes=[b_jk4, b_sq])
                rstd_from_ssq(sq[:, 0:1], sq[:, 1:2], sq[:, 2:3], 1024.0, [b_sq], [b_sq], via="act")
                cx.op("dve", lambda e: e.scalar_tensor_tensor(out=h2[:], in0=x1_[:], scalar=sq[:, 1:2], in1=a2rep[:],
                                                              op0=ALU.mult, op1=ALU.mult),
                      reads=[b_x1, b_sq, b_a2rep], writes=[b_h2])
                cx.op("dve", lambda e: e.tensor_tensor(out=h2[:], in0=h2[:], in1=sh2rep[:], op=ALU.add),
                      reads=[b_h2, b_sh2rep], writes=[b_h2])
                cx.op("act", lambda e: e.activation(out=hb[:], in_=h2[:], func=AF.Copy), reads=[b_h2], writes=[b_hb])
                cx.dma("sp", H2[rows, :], hb[:], reads=[b_hb])

            def stC2_1d(tt):
                h2, b_h2 = h2_2[tt % 2]
                lg, b_lg = lg_2[tt % 2]
                for kc in range(8):
                    ph, b_ph = psH[kc // 4]
                    cx.op("pe", lambda e: e.transpose(out=ph[:, (kc % 4) * 128:(kc % 4 + 1) * 128],
                                                      in_=h2[:, kc * 128:(kc + 1) * 128], identity=ident_f),
                          reads=[b_h2, b_cst], writes=[b_ph])
                cx.op("act", lambda e: e.activation(out=h2T[:, 0:4, :], in_=psH[0][0][:, :].rearrange("p (a b) -> p a b", a=4),
                                                    func=AF.Copy), reads=[psH[0][1]], writes=[b_h2T])
                cx.op("dve", lambda e: e.tensor_copy(out=h2T[:, 4:8, :], in_=psH[1][0][:, :].rearrange("p (a b) -> p a b", a=4)),
                      reads=[psH[1][1]], writes=[b_h2T])
                for kc in range(8):
                    cx.op("pe", lambda e: e.matmul(psL[0:36, 0:128], lhsT=wrt[:, kc, :], rhs=h2T[:, kc, :], start=(kc == 0), stop=(kc == 7)),
                          reads=[b_h2T, b_wrt], writes=[b_psL])
                cx.op("act", lambda e: e.activation(out=lgT[0:36, :], in_=psL[0:36, 0:128], func=AF.Copy), reads=[b_psL], writes=[b_lgT])
                cx.op("pe", lambda e: e.transpose(out=psL[:, 128:164], in_=lgT[0:36, :], identity=ident_f[0:36, 0:36]),
                      reads=[b_lgT, b_cst], writes=[b_psL])
                cx.op("dve", lambda e: e.tensor_tensor(out=lg[:], in0=psL[:, 128:164], in1=brr[:], op=ALU.add),
                      reads=[b_psL, b_brr], writes=[b_lg])
            D_ = lambda fn, r, w: cx.op("dve", fn, reads=r, writes=w)

            def stD1d(tt):
                lg, b_lg = lg_2[tt % 2]
                b_OH = b_OHt[tt]
                D_(lambda e: e.tensor_reduce(out=rt[:, 0:1], in_=lg[:, 0:4], axis=AX.X, op=ALU.max), [b_lg], [b_rt])
                D_(lambda e: e.tensor_scalar(out=rt[:, 1:2], in0=rt[:, 0:1], scalar1=-1.0, scalar2=None, op0=ALU.mult), [b_rt], [b_rt])
                cx.op("act", lambda e: e.activation(out=ge[:], in_=lg[:, 0:4], func=AF.Exp, bias=rt[:, 1:2], accum_out=rt[:, 2:3]),
                      reads=[b_lg, b_rt], writes=[b_ge, b_rt])
                D_(lambda e: e.reciprocal(out=rt[:, 3:4], in_=rt[:, 2:3]), [b_rt], [b_rt])
                D_(lambda e: e.tensor_scalar(out=gsel[:], in0=lg[:, 0:4], scalar1=rt[:, 0:1], scalar2=None, op0=ALU.is_equal),
                   [b_lg, b_rt], [b_gsel])
                D_(lambda e: e.tensor_copy(out=emask[:, :].rearrange("p (g e) -> p g e", g=4),
                                           in_=gsel[:, :].unsqueeze(2).to_broadcast([128, 4, 8])), [b_gsel], [b_emask])
                D_(lambda e: e.tensor_tensor(out=elm[:], in0=lg[:, 4:36], in1=emask[:], op=ALU.mult), [b_lg, b_emask], [b_elm])
                D_(lambda e: e.tensor_scalar(out=t32_[:], in0=emask[:], scalar1=-1.0, scalar2=1e9, op0=ALU.add, op1=ALU.mult),
                   [b_emask], [b_t32])
                D_(lambda e: e.tensor_tensor(out=elm[:], in0=elm[:], in1=t32_[:], op=ALU.add), [b_elm, b_t32], [b_elm])
                oh1 = OH[:, 2 * tt, :]
                oh2 = OH[:, 2 * tt + 1, :]
                D_(lambda e: e.tensor_reduce(out=rt[:, 4:5], in_=elm[:], axis=AX.X, op=ALU.max), [b_elm], [b_rt])
                D_(lambda e: e.tensor_scalar(out=oh1, in0=elm[:], scalar1=rt[:, 4:5], scalar2=None, op0=ALU.is_equal),
                   [b_elm, b_rt], [b_OH])
                D_(lambda e: e.scalar_tensor_tensor(out=elm[:], in0=oh1, scalar=-1e9, in1=elm[:], op0=ALU.mult, op1=ALU.add),
                   [b_OH, b_elm], [b_elm])
                D_(lambda e: e.tensor_reduce(out=rt[:, 5:6], in_=elm[:], axis=AX.X, op=ALU.max), [b_elm], [b_rt])
                D_(lambda e: e.tensor_scalar(out=oh2, in0=elm[:], scalar1=rt[:, 5:6], scalar2=None, op0=ALU.is_equal),
                   [b_elm, b_rt], [b_OH])
                D_(lambda e: e.tensor_scalar(out=rt[:, 6:7], in0=rt[:, 4:5], scalar1=-1.0, scalar2=None, op0=ALU.mult), [b_rt], [b_rt])
                cx.op("act", lambda e: e.activation(out=rt[:, 7:8], in_=rt[:, 5:6], func=AF.Exp, bias=rt[:, 6:7]),
                      reads=[b_rt], writes=[b_rt])
                D_(lambda e: e.tensor_scalar(out=rt[:, 8:9], in0=rt[:, 7:8], scalar1=1.0, scalar2=None, op0=ALU.add), [b_rt], [b_rt])
                D_(lambda e: e.reciprocal(out=rt[:, 9:10], in_=rt[:, 8:9]), [b_rt], [b_rt])
                D_(lambda e: e.tensor_tensor(out=WTS[:, tt, 0:1], in0=rt[:, 9:10], in1=rt[:, 3:4], op=ALU.mult), [b_rt], [b_WTS])
                D_(lambda e: e.tensor_tensor(out=WTS[:, tt, 1:2], in0=WTS[:, tt, 0:1], in1=rt[:, 7:8], op=ALU.mult),
                   [b_rt, b_WTS], [b_WTS])
                c32, b_c32 = cnt2[tt % 2]
                cb_, b_cb = cntb2[tt % 2]
                D_(lambda e: e.tensor_tensor(out=c32[:], in0=oh1, in1=oh2, op=ALU.add), [b_OH], [b_c32])
                D_(lambda e: e.tensor_copy(out=cb_[:], in_=c32[:]), [b_c32], [b_cb])

            def stDb1d(tt):
                b_OH = b_OHt[tt]
                c32, b_c32 = cnt2[tt % 2]
                cb_, b_cb = cntb2[tt % 2]
                cx.op("pe", lambda e: e.matmul(psR[:, 0:32], lhsT=US_b, rhs=cb_[:], start=True, stop=False),
                      reads=[b_cbf, b_cb], writes=[b_psR])
                cx.op("pe", lambda e: e.matmul(psR[:, 0:32], lhsT=ones_b, rhs=cntsb[:], start=False, stop=True),
                      reads=[b_cbf, b_cntsb], writes=[b_psR])
                for k in range(2):
                    ohk = OH[:, 2 * tt + k, :]
                    D_(lambda e: e.tensor_tensor(out=elmb[:], in0=psR[:, 0:32], in1=ohk, op=ALU.mult), [b_psR, b_OH, b_elmb], [b_elmb])
                    D_(lambda e: e.tensor_reduce(out=RK[:, 2 * tt + k:2 * tt + k + 1], in_=elmb[:], axis=AX.X, op=ALU.add),
                       [b_elmb], [b_RK])
                D_(lambda e: e.tensor_tensor(out=cnts[:], in0=cnts[:], in1=c32[:], op=ALU.add), [b_cnts, b_c32], [b_cnts])
                D_(lambda e: e.tensor_copy(out=cntsb[:], in_=cnts[:]), [b_cnts], [b_cntsb])
            ld1d(0)
            stC1d(0)
            if NT > 1:
                cx.zip_emit([lambda: stC1d(1), lambda: stC2_1d(0)])
            else:
                stC2_1d(0)
            for tt in range(NT):
                fns = []
                if tt + 2 < NT:
                    fns.append(lambda: stC1d(tt + 2))
                if tt + 1 < NT:
                    fns.append(lambda: stC2_1d(tt + 1))
                fns.append(lambda: stD1d(tt))
                if tt >= 1:
                    fns.append(lambda: stDb1d(tt - 1))
                cx.zip_emit(fns)
            stDb1d(NT - 1)
            cx.op("pe", lambda e: e.matmul(psR[:, 0:32], lhsT=ones_b, rhs=cntsb[:], start=True, stop=True),
                  reads=[b_cbf, b_cntsb], writes=[b_psR])
            D_(lambda e: e.tensor_copy(out=tot[:], in_=psR[:, 0:32]), [b_psR], [b_tot])
            cx.op("pool", lambda e: e.memset(pad[:], 0.0), writes=[b_pad])
            for m_ in range((2 * S) // BLK):
                D_(lambda e: e.scalar_tensor_tensor(out=pad[:], in0=tot[:], scalar=float(m_ * BLK), in1=pad[:],
                                                    op0=ALU.is_gt, op1=ALU.add), [b_tot, b_pad], [b_pad])
            D_(lambda e: e.tensor_scalar(out=pad[:], in0=pad[:], scalar1=float(BLK), scalar2=None, op0=ALU.mult), [b_pad], [b_pad])
            D_(lambda e: e.tensor_copy(out=pend[:, 0:1], in_=pad[:, 0:1]), [b_pad], [b_pend])
            for e_ in range(1, 32):
                D_(lambda e: e.tensor_tensor(out=pend[:, e_:e_ + 1], in0=pend[:, e_ - 1:e_], in1=pad[:, e_:e_ + 1], op=ALU.add),
                   [b_pad, b_pend], [b_pend])
            D_(lambda e: e.tensor_tensor(out=pstart[:], in0=pend[:], in1=pad[:], op=ALU.subtract), [b_pend, b_pad], [b_pstart])
            D_(lambda e: e.tensor_copy(out=dacc[:], in_=RK[:]), [b_RK], [b_dacc])
            for e_ in range(32):
                D_(lambda e: e.scalar_tensor_tensor(out=dacc[:], in0=OH[:, :, e_], scalar=pstart[:, e_:e_ + 1], in1=dacc[:],
                                                    op0=ALU.mult, op1=ALU.add), b_OHt + [b_pstart, b_dacc], [b_dacc])
            D_(lambda e: e.tensor_copy(out=DESTi[:].rearrange("p t k -> p (t k)"), in_=dacc[:]), [b_dacc], [b_DESTi])
            cx.op("pool", lambda e: e.memset(be[:], 0.0), writes=[b_be])
            for e_ in range(31):
                D_(lambda e: e.scalar_tensor_tensor(out=be[:], in0=bpos, scalar=pend[:, e_:e_ + 1], in1=be[:],
                                                    op0=ALU.is_ge, op1=ALU.add), [b_cst, b_pend, b_be], [b_be])
            same, b_same = TB(st, "same", [128, NB], F32)
            cx.op("pool", lambda e: e.memset(same[:], 0.0), writes=[b_same])
            D_(lambda e: e.tensor_tensor(out=same[:, 2:NB], in0=be[:, 2:NB], in1=be[:, 0:NB - 2], op=ALU.is_equal),
               [b_be, b_same], [b_same])
            D_(lambda e: e.tensor_scalar(out=be[:], in0=be[:], scalar1=128.0, scalar2=piota, op0=ALU.mult, op1=ALU.add),
               [b_be, b_cst], [b_be])
            D_(lambda e: e.scalar_tensor_tensor(out=be[:], in0=same[:], scalar=1048576.0, in1=be[:], op0=ALU.mult, op1=ALU.add),
               [b_same, b_be], [b_be])
            D_(lambda e: e.tensor_copy(out=widx[:], in_=be[:]), [b_be], [b_widx])
            if debug:
                cx.dma("sp", DBG[:, 0:2 * NT], dacc[:], reads=[b_dacc])
                cx.dma("sp", DBG[:, 512:512 + NB], be[:], reads=[b_be])
                cx.dma("sp", DBG[:, 1024:1024 + 2 * NT], WTS[:].rearrange("p t k -> p (t k)"), reads=[b_WTS])
                cx.dma("sp", DBG[:, 2048:2048 + 32], pend[:], reads=[b_pend])
            cx.barrier()

        with ExitStack() as st:
            hb = [TB(st, "hb1e%d" % i, [128, 1024], BF16) for i in range(4)]
            for tt in range(NT):
                t_, b_t = hb[tt % 4]
                cx.dma("sp", t_[:], H2[tt * 128:(tt + 1) * 128, :], writes=[b_t])
                for k in range(2):
                    cx.idma(out=HS, out_off=DESTi[:, tt, k:k + 1], in_=t_[:], in_off=None,
                            reads=[b_t, b_DESTi])
            cx.barrier()

        with ExitStack() as st:
            psT, b_psT = PB(st, "p2_T", [128, 8, 128], BF16)
            psA = [PB(st, "p2_A%d" % i, [128, 512], F32) for i in range(2)]
            psB = [PB(st, "p2_B%d" % i, [128, 512], F32) for i in range(2)]
            psY = [PB(st, "p2_Y%d" % i, [128, 512], F32) for i in range(2)]
            psT2, b_psT2 = PB(st, "p2_T2", [128, 4, 128], BF16)
            W1 = [TB(st, "W1_%d" % i, [128, 8, 512], BF16) for i in range(2)]
            W3 = [TB(st, "W3_%d" % i, [128, 8, 512], BF16) for i in range(2)]
            W2 = [TB(st, "W2_%d" % i, [128, 4, 1024], BF16) for i in range(2)]
            hs = [TB(st, "hs%d" % i, [128, 1024], BF16) for i in range(4)]
            yo = [TB(st, "yo%d" % i, [128, 1024], BF16) for i in range(2)]

            def load_w(b, which=(0, 1, 2)):
                for wi, (Wl, src) in enumerate(((W1, WB[0]), (W3, WB[1]), (W2, WB[2]))):
                    if wi not in which:
                        continue
                    w_, b_w = Wl[b % 2]
                    cx.idma(out=w_[:].rearrange("p a b -> p (a b)"), out_off=None, in_=src, in_off=widx[:, b:b + 1],
                            reads=[b_widx], writes=[b_w], bounds=wbound)

            g2rep, b_g2rep = TB(st, "g2rep2", [128, 1024], F32)
            cx.dma("sp", g2rep[:], REPS[2], writes=[b_g2rep])
            hT2 = [TB(st, "hT2_%d" % i, [128, 8, 128], BF16) for i in range(2)]
            sa2 = [TB(st, "sa2_%d" % i, [128, 512], F32) for i in range(2)]
            hid2 = [TB(st, "hid2_%d" % i, [128, 512], BF16) for i in range(2)]
            hidT2 = [TB(st, "hidT2_%d" % i, [128, 4, 128], BF16) for i in range(2)]
            NSUB = NB * (BLK // 128)
            SPB = BLK // 128

            def wts(n):
                b = n // SPB
                return W1[b % 2], W3[b % 2], W2[b % 2]

            def ldA(n):
                r0 = n * 128
                h_, b_h = hs[n % 4]
                cx.dma("sp", h_[:], HS[r0:r0 + 128, :], writes=[b_h])

            def stA(n):
                h_, b_h = hs[n % 4]
                t_, b_t = hT2[n % 2]
                for kc in range(8):
                    cx.op("pe", lambda e: e.transpose(out=psT[:, kc, :], in_=h_[:, kc * 128:(kc + 1) * 128], identity=ident_b),
                          reads=[b_h, b_cbf], writes=[b_psT])
                cx.op("act", lambda e: e.activation(out=t_[:, 0:4, :], in_=psT[:, 0:4, :], func=AF.Copy),
                      reads=[b_psT], writes=[b_t])
                cx.op("dve", lambda e: e.tensor_copy(out=t_[:, 4:8, :], in_=psT[:, 4:8, :]), reads=[b_psT], writes=[b_t])

            def stB(n):
                (w1_, b_w1), (w3_, b_w3), _ = wts(n)
                t_, b_t = hT2[n % 2]
                pa, b_pa = psA[n % 2]
                pb, b_pb = psB[n % 2]
                sa_, b_sa = sa2[n % 2]
                hd, b_hd = hid2[n % 2]
                for kc in range(8):
                    cx.op("pe", lambda e: e.matmul(pa[:, :], lhsT=t_[:, kc, :], rhs=w1_[:, kc, :], start=(kc == 0), stop=(kc == 7)),
                          reads=[b_t, b_w1], writes=[b_pa])
                for kc in range(8):
                    cx.op("pe", lambda e: e.matmul(pb[:, :], lhsT=t_[:, kc, :], rhs=w3_[:, kc, :], start=(kc == 0), stop=(kc == 7)),
                          reads=[b_t, b_w3], writes=[b_pb])
                cx.op("act", lambda e: e.activation(out=sa_[:], in_=pa[:, :], func=AF.Silu), reads=[b_pa], writes=[b_sa])
                cx.op("dve", lambda e: e.tensor_tensor(out=hd[:], in0=pb[:, :], in1=sa_[:], op=ALU.mult),
                      reads=[b_pb, b_sa], writes=[b_hd])

            def stC(n):
                hd, b_hd = hid2[n % 2]
                ht, b_ht = hidT2[n % 2]
                for fc in range(4):
                    cx.op("pe", lambda e: e.transpose(out=psT2[:, fc, :], in_=hd[:, fc * 128:(fc + 1) * 128], identity=ident_b),
                          reads=[b_hd, b_cbf], writes=[b_psT2])
                cx.op("act", lambda e: e.activation(out=ht[:, 0:2, :], in_=psT2[:, 0:2, :], func=AF.Copy),
                      reads=[b_psT2], writes=[b_ht])
                cx.op("dve", lambda e: e.tensor_copy(out=ht[:, 2:4, :], in_=psT2[:, 2:4, :]), reads=[b_psT2], writes=[b_ht])

            def stD(n):
                _, _, (w2_, b_w2) = wts(n)
                r0 = n * 128
                ht, b_ht = hidT2[n % 2]
                y_, b_y = yo[n % 2]
                for half in range(2):
                    py, b_py = psY[half]
                    for fc in range(4):
                        cx.op("pe", lambda e: e.matmul(py[:, :], lhsT=ht[:, fc, :], rhs=w2_[:, fc, half * 512:(half + 1) * 512],
                                                       start=(fc == 0), stop=(fc == 3)), reads=[b_ht, b_w2], writes=[b_py])
                    cx.op("dve", lambda e: e.tensor_tensor(out=y_[:, half * 512:(half + 1) * 512], in0=py[:, :],
                                                           in1=g2rep[:, half * 512:(half + 1) * 512], op=ALU.mult),
                          reads=[b_py, b_g2rep], writes=[b_y])
                cx.dma("sp", YS[r0:r0 + 128, :], y_[:], reads=[b_y])

            load_w(0)
            if NB > 1:
                load_w(1)
            ldA(0)
            ldA(1)
            ldA(2)
            stA(0)
            for n in range(NSUB + 1):
                if n + 3 < NSUB:
                    ldA(n + 3)
                if n + 1 < NSUB:
                    stA(n + 1)
                if n >= 1:
                    stC(n - 1)
                if n < NSUB:
                    stB(n)
                    if n % SPB == SPB - 1 and n // SPB + 2 < NB:
                        load_w(n // SPB + 2, which=(0, 1))
                if n >= 1:
                    stD(n - 1)
                    if (n - 1) % SPB == SPB - 1 and (n - 1) // SPB + 2 < NB:
                        load_w((n - 1) // SPB + 2, which=(2,))
            cx.barrier()

        with ExitStack() as st:
            ya = [TB(st, "ya3_%d" % i, [128, 1024], BF16) for i in range(4)]
            yb = [TB(st, "yb3_%d" % i, [128, 1024], BF16) for i in range(4)]
            x1 = [TB(st, "x13_%d" % i, [128, 1024], F32) for i in range(4)]
            m_ = [TB(st, "m3_%d" % i, [128, 1024], F32) for i in range(2)]
            o_ = [TB(st, "o3_%d" % i, [128, 1024], F32) for i in range(2)]
            jk5, b_jk5 = TB(st, "jk5", [128, 1024], BF16)
            fngr, b_fngr = TB(st, "fngr3", [128, 1024], F32)
            cx.dma("sp", fngr[:], fng_rep, writes=[b_fngr])
            sq = [TB(st, "sq3_%d" % i, [128, 4], F32) for i in range(2)]

            def ld3(tt):
                rows = slice(tt * 128, (tt + 1) * 128)
                a_, b_a = ya[tt % 4]
                bb_, b_b = yb[tt % 4]
                x_, b_x = x1[tt % 4]
                cx.idma(out=a_[:], out_off=None, in_=YS, in_off=DESTi[:, tt, 0:1], reads=[b_DESTi], writes=[b_a])
                cx.idma(out=bb_[:], out_off=None, in_=YS, in_off=DESTi[:, tt, 1:2], reads=[b_DESTi], writes=[b_b])
                cx.dma("sp", x_[:], X1[rows, :], writes=[b_x])

            jk5b = [(jk5, b_jk5), TB(st, "jk5b", [128, 1024], BF16)]

            def p3_tile(tt):
                rows = slice(tt * 128, (tt + 1) * 128)
                a_, b_a = ya[tt % 4]
                bb_, b_b = yb[tt % 4]
                x_, b_x = x1[tt % 4]
                mm_, b_m = m_[tt % 2]
                oo_, b_o = o_[tt % 2]
                s_, b_s = sq[tt % 2]
                jk_, b_jk = jk5b[tt % 2]
                cx.op("act", lambda e: e.activation(out=mm_[:], in_=a_[:], func=AF.Copy, scale=WTS[:, tt, 0:1]),
                      reads=[b_a, b_WTS], writes=[b_m])
                cx.op("dve", lambda e: e.scalar_tensor_tensor(out=mm_[:], in0=bb_[:], scalar=WTS[:, tt, 1:2], in1=mm_[:],
                                                              op0=ALU.mult, op1=ALU.add), reads=[b_b, b_WTS, b_m], writes=[b_m])
                cx.op("dve", lambda e: e.tensor_tensor(out=mm_[:], in0=mm_[:], in1=x_[:], op=ALU.add), reads=[b_m, b_x], writes=[b_m])
                cx.op("act", lambda e: e.activation(out=jk_[:], in_=mm_[:], func=AF.Square, accum_out=s_[:, 0:1]),
                      reads=[b_m], writes=[b_jk, b_s])
                rstd_from_ssq(s_[:, 0:1], s_[:, 1:2], s_[:, 2:3], 1024.0, [b_s], [b_s], via="act")
                cx.op("dve", lambda e: e.scalar_tensor_tensor(out=oo_[:], in0=mm_[:], scalar=s_[:, 1:2], in1=fngr[:],
                                                              op0=ALU.mult, op1=ALU.mult), reads=[b_m, b_s, b_fngr], writes=[b_o])
                cx.dma("sp", out[rows, :], oo_[:], reads=[b_o])

            ld3(0)
            if NT > 1:
                ld3(1)
            for tt in range(0, NT, 2):
                for k in (2, 3):
                    if tt + k < NT:
                        ld3(tt + k)
                if tt + 1 < NT:
                    cx.zip_emit([lambda: p3_tile(tt), lambda: p3_tile(tt + 1)])
                else:
                    p3_tile(tt)
            cx.barrier()
    return nc


def _prep_shared(inputs, NB):
    f = lambda a: np.ascontiguousarray(np.asarray(a, dtype=np.float32))
    rep = lambda v: f(np.broadcast_to(np.asarray(v, np.float32).reshape(1, -1), (128, np.asarray(v).size)))
    col = lambda v: f(np.asarray(v, np.float32).reshape(8, 128).T)
    d = {}
    d["w_ada"] = f(inputs["w_ada"][0])
    d["b_ada"] = f(inputs["b_ada"][0].reshape(1, -1))
    d["n1g_col"] = col(inputs["norm1_g"][0])
    d["n2g_rep"] = rep(inputs["norm2_g"][0])
    d["fng_rep"] = rep(inputs["final_norm_g"])
    d["w_in"] = f(inputs["w_in"][0])
    d["lamv"] = rep(np.concatenate([np.asarray(inputs[k][0]) for k in
                                    ("attn_lambda_q1", "attn_lambda_k1", "attn_lambda_q2", "attn_lambda_k2")]))
    d["sg_rep"] = rep(inputs["attn_subln_g"][0])
    d["tb_rep"] = rep(np.asarray(inputs["rel_bias_table"]).reshape(-1))
    d["rlb_rep"] = rep(np.asarray(inputs["rec_lower_bound"]).reshape(-1))
    d["rng_rep"] = rep(inputs["rec_norm_g"][0])
    d["w_out"] = f(inputs["w_out"][0])
    wr = np.concatenate([np.asarray(inputs["w_group"][0]), np.asarray(inputs["w_expert"][0])], axis=1)
    d["wr"] = f(wr.reshape(8, 128, 36).transpose(1, 0, 2))
    d["br_rep"] = rep(np.concatenate([np.asarray(inputs["b_group"][0]), np.asarray(inputs["b_expert"][0])]))
    w1 = np.asarray(inputs["w1"][0], np.float32)
    w3 = np.asarray(inputs["w3"][0], np.float32)
    w2 = np.asarray(inputs["w2"][0], np.float32)
    d["w1r"] = f(w1.reshape(32, 8, 128, 512).transpose(0, 2, 1, 3).reshape(32 * 128, 4096))
    d["w3r"] = f(w3.reshape(32, 8, 128, 512).transpose(0, 2, 1, 3).reshape(32 * 128, 4096))
    d["w2r"] = f(w2.reshape(32, 4, 128, 1024).transpose(0, 2, 1, 3).reshape(32 * 128, 4096))
    d["consts"] = _consts(NB)
    return d


def run(inputs, S, nb, debug=False):
    NB = (2 * S) // BLK + 32
    nc = build(S, debug)
    shared = _prep_shared(inputs, NB)
    x = np.asarray(inputs["x"], np.float32)
    c = np.asarray(inputs["c"], np.float32)
    in_maps = []
    for b in range(nb):
        m = dict(shared)
        m["x"] = np.ascontiguousarray(x[b, :S])
        m["c_col"] = np.ascontiguousarray(c[b].reshape(8, 128).T)
        in_maps.append(m)
    res = run_bass_kernel_spmd(nc, in_maps, core_ids=list(range(nb)))
    return res


def kernel(**inputs):
    S = 8192
    res = run(inputs, S, 8)
    return np.stack([np.asarray(r["out"], np.float32) for r in res.results], axis=0)
```

```python
import math
import threading
import numpy as np
import ml_dtypes
from contextlib import ExitStack
import concourse.bass as bass
import concourse.mybir as mybir
from concourse.bass_utils import run_bass_kernel_spmd

F32 = mybir.dt.float32
BF16 = mybir.dt.bfloat16
I32 = mybir.dt.int32
AF = mybir.ActivationFunctionType
ALU = mybir.AluOpType
AX = mybir.AxisListType


class Buf:
    __slots__ = ("w", "r")

    def __init__(self):
        self.w = {}
        self.r = {}


class Ctx:
    NQ = 12

    def __init__(self, nc, st):
        self.nc = nc
        self.eng = {"pe": nc.tensor, "act": nc.scalar, "dve": nc.vector, "pool": nc.gpsimd, "sp": nc.sync}
        self.csem = {k: st.enter_context(nc.semaphore("c_" + k)) for k in ("pe", "act", "dve", "pool")}
        self.ccnt = {k: 0 for k in self.csem}
        self.dsem = {q: [st.enter_context(nc.semaphore("d_%s%d" % (q, i))) for i in range(self.NQ)]
                     for q in ("sp", "act", "pool")}
        self.dcnt = {q: [0] * self.NQ for q in self.dsem}
        self.drr = {q: 0 for q in self.dsem}
        self.known = {e: {} for e in self.eng}
        self._workers = {}

    class _Worker:
        def __init__(self, fn):
            self.fn = fn
            self.go = threading.Semaphore(0)
            self.back = threading.Semaphore(0)
            self.done = False
            self.exc = None
            self.th = threading.Thread(target=self._run, daemon=True)

        def _run(self):
            self.go.acquire()
            try:
                self.fn()
            except BaseException as e:
                self.exc = e
            self.done = True
            self.back.release()

        def gate(self):
            self.back.release()
            self.go.acquire()

        def step(self):
            self.go.release()
            self.back.acquire()

    def zip_emit(self, fns, weights=None):
        ws = [Ctx._Worker(f) for f in fns]
        if weights is None:
            weights = [1] * len(ws)
        for w in ws:
            self._workers[w.th] = w
            w.th.start()
        while not all(w.done for w in ws):
            for w, k in zip(ws, weights):
                for _ in range(k):
                    if not w.done:
                        w.step()
        for w in ws:
            del self._workers[w.th]
            if w.exc is not None:
                raise w.exc

    def _gate(self):
        w = self._workers.get(threading.current_thread())
        if w is not None:
            w.gate()

    def _sem(self, key):
        if key[0] == "c":
            return self.csem[key[1]]
        return self.dsem[key[1]][key[2]]

    def _wait(self, en, evs):
        kn = self.known[en]
        for key, val in evs.items():
            if key == ("c", "pe") and en == "pe":
                continue
            if kn.get(key, 0) < val:
                self.eng[en].wait_ge(self._sem(key), val)
                kn[key] = val

    @staticmethod
    def _deps(reads, writes):
        evs = {}
        for b in reads:
            for k, v in b.w.items():
                if evs.get(k, 0) < v:
                    evs[k] = v
        for b in writes:
            for d in (b.w, b.r):
                for k, v in d.items():
                    if evs.get(k, 0) < v:
                        evs[k] = v
        return evs

    @staticmethod
    def _record(ev, reads, writes):
        k, v = ev
        for b in reads:
            b.r[k] = v
        for b in writes:
            b.w = {k: v}
            b.r = {}

    def op(self, en, fn, reads=(), writes=()):
        self._gate()
        self._wait(en, self._deps(reads, writes))
        inst = fn(self.eng[en])
        self.ccnt[en] += 1
        inst.then_inc(self.csem[en], 1)
        self._record((("c", en), self.ccnt[en]), reads, writes)
        return inst

    def _dma_common(self, q, reads, writes, issue):
        self._gate()
        i = self.drr[q]
        self.drr[q] = (i + 1) % self.NQ
        evs = self._deps(reads, writes)
        prev = self.dcnt[q][i]
        if prev > 0:
            evs[("d", q, i)] = prev
        self._wait(q, evs)
        inst = issue(self.eng[q])
        self.dcnt[q][i] += 16
        inst.then_inc(self.dsem[q][i], 16)
        self._record((("d", q, i), self.dcnt[q][i]), reads, writes)
        return inst

    def dma(self, q, out, in_, reads=(), writes=(), **kw):
        return self._dma_common(q, reads, writes, lambda e: e.dma_start(out=out, in_=in_, **kw))

    def idma(self, out, out_off, in_, in_off, reads=(), writes=(), bounds=None):
        def issue(e):
            oo = bass.IndirectOffsetOnAxis(ap=out_off, axis=0) if out_off is not None else None
            io = bass.IndirectOffsetOnAxis(ap=in_off, axis=0) if in_off is not None else None
            if bounds is not None:
                return e.indirect_dma_start(out=out, out_offset=oo, in_=in_, in_offset=io,
                                            bounds_check=bounds, oob_is_err=False)
            return e.indirect_dma_start(out=out, out_offset=oo, in_=in_, in_offset=io)
        return self._dma_common("pool", reads, writes, issue)

    def barrier(self):
        evs = {}
        for k, c in self.ccnt.items():
            if c > 0:
                evs[("c", k)] = c
        for q in self.dsem:
            for i, c in enumerate(self.dcnt[q]):
                if c > 0:
                    evs[("d", q, i)] = c
        for en in self.eng:
            kn = self.known[en]
            for key, val in evs.items():
                if kn.get(key, 0) < val:
                    self.eng[en].wait_ge(self._sem(key), val)
                    kn[key] = val


EPS = 1e-6
LAM_INIT = 0.8 - 0.6 * math.exp(-0.3 * 0)
BLK = 256


def _t5_bucket_np(rel):
    n = np.maximum(rel, 0)
    nf = np.maximum(n, 1).astype(np.float32)
    large = 16 + (np.log(nf / np.float32(16)) / np.float32(math.log(8.0)) * np.float32(16)).astype(np.int32)
    large = np.minimum(large, 31)
    return np.where(n < 16, n, large)


def _consts(NB):
    p = np.arange(128)[:, None]
    j = np.arange(128)[None, :]
    cols = []
    cols.append((p == j))
    cols.append((p <= j).astype(np.float32) - (p <= 63))
    cols.append((p > j))
    cols.append((p <= j))
    cols.append((p < j))
    cols.append(np.ones((128, 128)))
    rel0 = j - p
    cols.append(np.where(rel0 >= 0, _t5_bucket_np(rel0), -1))
    cols.append(_t5_bucket_np(128 + j - p))
    cols.append(np.arange(128)[:, None])
    cols.append(np.broadcast_to(np.arange(NB)[None, :] * BLK, (128, NB)))
    cols.append(np.concatenate([np.ones((128, 1)), (p <= 63)], axis=1))
    return np.concatenate([np.asarray(c, np.float32) for c in cols], axis=1)


def build(S, debug=False):
    NT = S // 128
    NJ = S // 512
    NB = (2 * S) // BLK + 32
    NROW = NB * BLK
    NC = 1025 + NB + 2
    nc = bass.Bass("TRN2", target_bir_lowering=False)

    def din(name, shape, dt=F32):
        return nc.dram_tensor(name, shape, dt, kind="ExternalInput").ap()

    x = din("x", [S, 1024])
    c_col = din("c_col", [128, 8])
    w_ada = din("w_ada", [1024, 6144])
    b_ada = din("b_ada", [1, 6144])
    n1g_col = din("n1g_col", [128, 8])
    n2g_rep = din("n2g_rep", [128, 1024])
    fng_rep = din("fng_rep", [128, 1024])
    w_in = din("w_in", [1024, 3584])
    lamv = din("lamv", [128, 256])
    sg_rep = din("sg_rep", [128, 128])
    tb_rep = din("tb_rep", [128, 128])
    rlb_rep = din("rlb_rep", [128, 1024])
    rng_rep = din("rng_rep", [128, 512])
    w_out = din("w_out", [1024, 1024])
    wr = din("wr", [128, 8, 36])
    br_rep = din("br_rep", [128, 36])
    w1r = din("w1r", [32 * 128, 4096])
    w3r = din("w3r", [32 * 128, 4096])
    w2r = din("w2r", [32 * 128, 4096])
    consts = din("consts", [128, NC])
    out = nc.dram_tensor("out", [S, 1024], F32, kind="ExternalOutput").ap()
    skind = "ExternalOutput" if debug else "Internal"

    def dscr(name, shape, dt):
        return nc.dram_tensor(name, shape, dt, kind=skind).ap()

    WP = dscr("WP", [1024, 3584], BF16)
    WOP = dscr("WOP", [1024, 1024], BF16)
    QT = dscr("QT", [4, 128, S], BF16)
    QR = dscr("QR", [S, 512], BF16)
    FR = dscr("FR", [S, 512], F32)
    IR = dscr("IR", [S, 512], BF16)
    GR = dscr("GR", [S, 512], BF16)
    MIX = dscr("MIX", [S, 1024], BF16)
    X1 = dscr("X1", [S, 1024], F32)
    H2 = dscr("H2", [S, 1024], BF16)
    HS = dscr("HS", [NROW, 1024], BF16)
    REPS = dscr("REPS", [3, 128, 1024], F32)
    WB = [dscr("W%dB" % i, [32 * 128, 4096], BF16) for i in range(3)]
    YS = dscr("YS", [NROW, 1024], BF16)
    if debug:
        DBG = nc.dram_tensor("DBG", [128, 4096], F32, kind="ExternalOutput").ap()

    kcp = lambda ap: ap.rearrange("(kc p) n -> p kc n", p=128)

    with ExitStack() as top:
        cx = Ctx(nc, top)
        wbound = nc.gpsimd.alloc_register("wbound")
        nc.gpsimd.reg_mov(wbound, 32 * 128 - 1)

        def TB(st, name, shape, dt):
            return st.enter_context(nc.sbuf_tensor(name, shape, dt)), Buf()

        def PB(st, name, shape, dt):
            return st.enter_context(nc.psum_tensor(name, shape, dt)), Buf()

        class Rot:
            def __init__(self, items):
                self.items = items
                self.i = 0

            def next(self):
                it = self.items[self.i % len(self.items)]
                self.i += 1
                return it

        cst, b_cst = TB(top, "cst", [128, NC], F32)
        ident_f = cst[:, 0:128]
        L1 = cst[:, 128:256]
        L2 = cst[:, 256:384]
        MT = cst[:, 384:512]
        US_f = cst[:, 512:640]
        ones_f = cst[:, 640:768]
        idxD = [cst[:, 768:896], cst[:, 896:1024]]
        piota = cst[:, 1024:1025]
        bpos = cst[:, 1025:1025 + NB]
        sel2 = cst[:, 1025 + NB:1027 + NB]
        cbf, b_cbf = TB(top, "cbf", [128, 384], BF16)
        ident_b = cbf[:, 0:128]
        US_b = cbf[:, 128:256]
        ones_b = cbf[:, 256:384]
        neglam, b_neglam = TB(top, "neglam", [128, 1], F32)
        negb31, b_negb31 = TB(top, "negb31", [128, 4], F32)
        tbr, b_tbr = TB(top, "tbr", [128, 128], F32)
        EB, b_EB = TB(top, "EB", [128, 8, 128], BF16)
        sgr, b_sgr = TB(top, "sgr", [128, 128], F32)
        biascol, b_biascol = TB(top, "biascol", [128, 8], F32)
        bias_hl, b_bias_hl = TB(top, "bias_hl", [33, 3584], BF16)
        WTS, b_WTS = TB(top, "WTS", [128, NT, 2], F32)
        DESTi, b_DESTi = TB(top, "DESTi", [128, NT, 2], I32)
        widx, b_widx = TB(top, "widx", [128, NB], I32)

        cx.dma("sp", cst[:], consts, writes=[b_cst])
        cx.dma("sp", tbr[:], tb_rep, writes=[b_tbr])
        cx.dma("sp", sgr[:], sg_rep, writes=[b_sgr])
        cx.op("dve", lambda e: e.tensor_copy(out=cbf[:, 0:128], in_=ident_f), reads=[b_cst], writes=[b_cbf])
        cx.op("dve", lambda e: e.tensor_copy(out=cbf[:, 128:256], in_=US_f), reads=[b_cst], writes=[b_cbf])
        cx.op("dve", lambda e: e.tensor_copy(out=cbf[:, 256:384], in_=ones_f), reads=[b_cst], writes=[b_cbf])

        mhalf, b_mhalf = TB(top, "mhalf", [128, 8], F32)
        cx.op("pool", lambda e: e.memset(mhalf[:], -0.5), writes=[b_mhalf])

        def rstd_from_ssq(ssq_ap, out_ap, tmp_ap, n, rbufs, wbufs, via="pool"):
            w = ssq_ap.shape[-1]
            cx.op("dve", lambda e: e.tensor_scalar(out=tmp_ap, in0=ssq_ap, scalar1=1.0 / n, scalar2=EPS,
                                                   op0=ALU.mult, op1=ALU.add), reads=rbufs, writes=wbufs)
            if via == "act":
                cx.op("act", lambda e: e.activation(out=tmp_ap, in_=tmp_ap, func=AF.Ln), reads=wbufs, writes=wbufs)
                cx.op("act", lambda e: e.activation(out=out_ap, in_=tmp_ap, func=AF.Exp, scale=-0.5),
                      reads=wbufs, writes=wbufs)
                return
            cx.op("pool", lambda e: e.tensor_tensor(out=out_ap, in0=tmp_ap, in1=mhalf[:, 0:w], op=ALU.pow),
                  reads=list(wbufs) + [b_mhalf], writes=wbufs)

        with ExitStack() as st:
            psb = [PB(st, "p0_%d" % i, [128, 512], F32) for i in range(4)]
            prot = Rot(psb)
            g2rep, b_g2rep = TB(st, "g2rep", [128, 1024], F32)
            a2rep, b_a2rep = TB(st, "a2rep", [128, 1024], F32)
            sh2rep, b_sh2rep = TB(st, "sh2rep", [128, 1024], F32)
            ccol, b_ccol = TB(st, "ccol", [128, 8], F32)
            cact, b_cact = TB(st, "cact", [128, 8], F32)
            badar, b_badar = TB(st, "badar", [1, 6144], F32)
            modr, b_modr = TB(st, "modr", [1, 6144], F32)
            n1gc, b_n1gc = TB(st, "n1gc", [128, 8], F32)
            sh1c, b_sh1c = TB(st, "sh1c", [128, 8], F32)
            s1c, b_s1c = TB(st, "s1c", [128, 8], F32)
            g1rep, b_g1rep = TB(st, "g1rep", [128, 1024], F32)
            n2gr, b_n2gr = TB(st, "n2gr", [128, 1024], F32)
            biasr, b_biasr = TB(st, "biasr", [33, 3584], F32)
            bt32, b_bt32 = TB(st, "bt32", [33, 3584], F32)
            bh32, b_bh32 = TB(st, "bh32", [33, 3584], BF16)
            sh1rep, b_sh1rep = TB(st, "sh1rep", [128, 8, 33], F32)
            wbig = [TB(st, "wbig%d" % i, [128, 8, 512], F32) for i in range(3)]
            wcast = [TB(st, "wcast%d" % i, [128, 8, 512], BF16) for i in range(3)]
            lv, b_lv = TB(st, "lv", [128, 256], F32)
            ltmp, b_ltmp = TB(st, "ltmp", [128, 128], F32)
            lsum, b_lsum = TB(st, "lsum", [128, 2], F32)
            eq, b_eq = TB(st, "eq", [128, 128], F32)
            bacc = [TB(st, "bacc%d" % h, [128, 128], F32) for h in range(4)]

            cx.dma("sp", ccol[:], c_col, writes=[b_ccol])
            cx.dma("sp", badar[:], b_ada, writes=[b_badar])
            cx.dma("sp", n1gc[:], n1g_col, writes=[b_n1gc])
            cx.dma("sp", n2gr[:], n2g_rep, writes=[b_n2gr])
            cx.dma("sp", lv[:], lamv, writes=[b_lv])
            cx.op("act", lambda e: e.activation(out=cact[:], in_=ccol[:], func=AF.Silu), reads=[b_ccol], writes=[b_cact])
            def p0_main():
                wq = ["sp", "act"]
                for g in range(12):
                    wt, b_wt = wbig[g % 3]
                    cx.dma(wq[g % 2], wt[:], kcp(w_ada)[:, :, g * 512:(g + 1) * 512], writes=[b_wt])
                    ps, b_ps = prot.next()
                    for kc in range(8):
                        cx.op("pe", lambda e: e.matmul(ps[0:1, :], lhsT=cact[:, kc:kc + 1], rhs=wt[:, kc, :],
                                                       start=(kc == 0), stop=(kc == 7)),
                              reads=[b_cact, b_wt], writes=[b_ps])
                    cx.op("dve", lambda e: e.tensor_tensor(out=modr[0:1, g * 512:(g + 1) * 512], in0=ps[0:1, :],
                                                           in1=badar[0:1, g * 512:(g + 1) * 512], op=ALU.add),
                          reads=[b_ps, b_badar], writes=[b_modr])
                ps, b_ps = prot.next()
                for jn in range(16):
                    off = (0 if jn < 8 else 1024) + (jn % 8) * 128
                    cx.op("pe", lambda e: e.matmul(ps[:, jn:jn + 1], lhsT=modr[0:1, off:off + 128], rhs=ones_f[0:1, 0:1],
                                                   start=True, stop=True), reads=[b_modr, b_cst], writes=[b_ps])
                cx.op("dve", lambda e: e.tensor_copy(out=sh1c[:], in_=ps[:, 0:8]), reads=[b_ps], writes=[b_sh1c])
                cx.op("dve", lambda e: e.scalar_tensor_tensor(out=s1c[:], in0=ps[:, 8:16], scalar=1.0, in1=n1gc[:],
                                                              op0=ALU.add, op1=ALU.mult),
                      reads=[b_ps, b_n1gc], writes=[b_s1c])
                for (dst, b_dst, off) in ((g1rep, b_g1rep, 2048), (sh2rep, b_sh2rep, 3072),
                                          (a2rep, b_a2rep, 4096), (g2rep, b_g2rep, 5120)):
                    for half in range(2):
                        ps, b_ps = prot.next()
                        cx.op("pe", lambda e: e.matmul(ps[:, :], lhsT=ones_f[0:1, 0:128],
                                                       rhs=modr[0:1, off + half * 512:off + (half + 1) * 512],
                                                       start=True, stop=True), reads=[b_modr, b_cst], writes=[b_ps])
                        cx.op("dve", lambda e: e.tensor_copy(out=dst[:, half * 512:(half + 1) * 512], in_=ps[:, :]),
                              reads=[b_ps], writes=[b_dst])
                cx.op("dve", lambda e: e.scalar_tensor_tensor(out=a2rep[:], in0=a2rep[:], scalar=1.0, in1=n2gr[:],
                                                              op0=ALU.add, op1=ALU.mult),
                      reads=[b_n2gr, b_a2rep], writes=[b_a2rep])
                cx.dma("sp", REPS[0], a2rep[:], reads=[b_a2rep])
                cx.dma("sp", REPS[1], sh2rep[:], reads=[b_sh2rep])
                cx.dma("sp", REPS[2], g2rep[:], reads=[b_g2rep])
                for kc in range(8):
                    cx.op("dve", lambda e: e.tensor_scalar(out=sh1rep[:, kc, :], in0=ones_f[:, 0:33], scalar1=sh1c[:, kc:kc + 1],
                                                           scalar2=None, op0=ALU.mult), reads=[b_cst, b_sh1c], writes=[b_sh1rep])
                cx.op("pool", lambda e: e.memset(bias_hl[:], 0.0), writes=[b_bias_hl])
                psbc, b_psbc = prot.next()
                engs3 = ["act", "dve", "dve"]
                for g in range(7):
                    wt, b_wt = wbig[g % 3]
                    cx.dma(wq[g % 2], wt[:], kcp(w_in)[:, :, g * 512:(g + 1) * 512], writes=[b_wt])
                    ps, b_ps = prot.next()
                    if ps is psbc:
                        ps, b_ps = prot.next()
                    for kc in range(8):
                        cx.op("pe", lambda e: e.matmul(ps[0:33, :], lhsT=sh1rep[:, kc, :], rhs=wt[:, kc, :],
                                                       start=(kc == 0), stop=(kc == 7)),
                              reads=[b_sh1rep, b_wt], writes=[b_ps])
                    cx.op("dve", lambda e: e.tensor_copy(out=biasr[:, g * 512:(g + 1) * 512], in_=ps[0:33, :]),
                          reads=[b_ps], writes=[b_biasr])
                    if g < 2:
                        for sub in range(4):
                            for kc in range(8):
                                cx.op("pe", lambda e: e.matmul(psbc[:, g * 4 + sub:g * 4 + sub + 1],
                                                               lhsT=wt[:, kc, sub * 128:(sub + 1) * 128],
                                                               rhs=sh1c[:, kc:kc + 1], start=(kc == 0), stop=(kc == 7)),
                                      reads=[b_sh1c, b_wt], writes=[b_psbc])
                    wc, b_wc = wcast[g % 3]
                    for kc in range(8):
                        en = engs3[kc % 3]
                        if en == "act":
                            cx.op("act", lambda e: e.activation(out=wc[:, kc, :], in_=wt[:, kc, :], func=AF.Copy,
                                                                scale=s1c[:, kc:kc + 1]),
                                  reads=[b_wt, b_s1c], writes=[b_wc])
                        else:
                            cx.op(en, lambda e: e.tensor_scalar(out=wc[:, kc, :], in0=wt[:, kc, :],
                                                                scalar1=s1c[:, kc:kc + 1], scalar2=None, op0=ALU.mult),
                                  reads=[b_wt, b_s1c], writes=[b_wc])
                    cx.dma("sp", kcp(WP)[:, :, g * 512:(g + 1) * 512], wc[:], reads=[b_wc])
                cx.op("dve", lambda e: e.tensor_copy(out=biascol[:], in_=psbc[:, 0:8]), reads=[b_psbc], writes=[b_biascol])
                cx.op("dve", lambda e: e.tensor_copy(out=bias_hl[0:1, :], in_=biasr[0:1, :]), reads=[b_biasr], writes=[b_bias_hl])
                cx.op("dve", lambda e: e.tensor_copy(out=bh32[32:33, :], in_=biasr[32:33, :]), reads=[b_biasr], writes=[b_bh32])
                cx.op("dve", lambda e: e.tensor_copy(out=bt32[32:33, :], in_=bh32[32:33, :]), reads=[b_bh32], writes=[b_bt32])
                cx.op("dve", lambda e: e.tensor_tensor(out=bt32[32:33, :], in0=biasr[32:33, :], in1=bt32[32:33, :], op=ALU.subtract),
                      reads=[b_biasr, b_bt32], writes=[b_bt32])
                cx.op("dve", lambda e: e.tensor_copy(out=bias_hl[32:33, :], in_=bt32[32:33, :]), reads=[b_bt32, b_bias_hl], writes=[b_bias_hl])
                for half in range(2):
                    wt, b_wt = wbig[(7 + half) % 3]
                    cx.dma(wq[half], wt[:], kcp(w_out)[:, :, half * 512:(half + 1) * 512], writes=[b_wt])
                    wc, b_wc = wcast[(7 + half) % 3]
                    for kc in range(8):
                        en = "dve"
                        cx.op(en, lambda e: e.tensor_tensor(out=wc[:, kc, :], in0=wt[:, kc, :],
                                                            in1=g1rep[:, half * 512:(half + 1) * 512], op=ALU.mult),
                              reads=[b_wt, b_g1rep], writes=[b_wc])
                    cx.dma("sp", kcp(WOP)[:, :, half * 512:(half + 1) * 512], wc[:], reads=[b_wc])

            def p0_misc():
                for i2 in range(2):
                    cx.op("dve", lambda e: e.tensor_tensor(out=ltmp[:, 0:64], in0=lv[:, i2 * 128:i2 * 128 + 64],
                                                           in1=lv[:, i2 * 128 + 64:i2 * 128 + 128], op=ALU.mult),
                          reads=[b_lv], writes=[b_ltmp])
                    cx.op("dve", lambda e: e.tensor_reduce(out=lsum[:, i2:i2 + 1], in_=ltmp[:, 0:64], axis=AX.X, op=ALU.add),
                          reads=[b_ltmp], writes=[b_lsum])
                cx.op("act", lambda e: e.activation(out=lsum[:], in_=lsum[:], func=AF.Exp), reads=[b_lsum], writes=[b_lsum])
                cx.op("dve", lambda e: e.scalar_tensor_tensor(out=neglam[:], in0=lsum[:, 1:2], scalar=-LAM_INIT,
                                                              in1=lsum[:, 0:1], op0=ALU.add, op1=ALU.subtract),
                      reads=[b_lsum], writes=[b_neglam])
                cx.op("dve", lambda e: e.tensor_scalar(out=sgr[:], in0=sgr[:], scalar1=1.0 - LAM_INIT, scalar2=None,
                                                       op0=ALU.mult), reads=[b_sgr], writes=[b_sgr])
                cx.op("dve", lambda e: e.tensor_scalar(out=negb31[:], in0=tbr[:, 124:128], scalar1=-1.0, scalar2=None,
                                                       op0=ALU.mult), reads=[b_tbr], writes=[b_negb31])
                for D in range(2):
                    for h in range(4):
                        cx.op("pool", lambda e: e.memset(bacc[h][0][:], 0.0), writes=[bacc[h][1]])
                    for b in range(32):
                        cx.op("dve", lambda e: e.tensor_scalar(out=eq[:], in0=idxD[D], scalar1=float(b), scalar2=None,
                                                               op0=ALU.is_equal), reads=[b_cst], writes=[b_eq])
                        for h in range(4):
                            en = "dve"
                            cx.op(en, lambda e: e.scalar_tensor_tensor(out=bacc[h][0][:], in0=eq[:],
                                                                       scalar=tbr[:, b * 4 + h:b * 4 + h + 1],
                                                                       in1=bacc[h][0][:], op0=ALU.mult, op1=ALU.add),
                                  reads=[b_eq, b_tbr, bacc[h][1]], writes=[bacc[h][1]])
                    for h in range(4):
                        cx.op("act", lambda e: e.activation(out=bacc[h][0][:], in_=bacc[h][0][:], func=AF.Exp,
                                                            bias=negb31[:, h:h + 1]),
                              reads=[bacc[h][1], b_negb31], writes=[bacc[h][1]])
                        if D == 0:
                            cx.op("dve", lambda e: e.tensor_tensor(out=EB[:, h * 2 + D, :], in0=bacc[h][0][:], in1=MT,
                                                                   op=ALU.mult), reads=[bacc[h][1], b_cst], writes=[b_EB])
                        else:
                            cx.op("dve", lambda e: e.tensor_copy(out=EB[:, h * 2 + D, :], in_=bacc[h][0][:]),
                                  reads=[bacc[h][1]], writes=[b_EB])

            cx.zip_emit([p0_main, p0_misc], weights=[1, 1])
            cx.barrier()

        with ExitStack() as stkv:
            KT = stkv.enter_context(nc.sbuf_tensor("KT", [128, 4, S], BF16))
            b_KT = [Buf() for _ in range(NJ)]
            VA = stkv.enter_context(nc.sbuf_tensor("VA", [128, NT, 4, 129], BF16))
            b_VA = [Buf() for _ in range(NT)]
            for tt in range(NT):
                cx.op("pool", lambda e: e.memset(VA[:, tt, :, 128:129], 1.0), writes=[b_VA[tt]])

            with ExitStack() as st:
                psf = [PB(st, "p1a_%d" % i, [128, 512], F32) for i in range(6)]
                prot = Rot(psf)
                pst = [PB(st, "p1aT_%d" % i, [128, 8, 128], BF16) for i in range(2)]
                xin = [TB(st, "xin%d" % i, [128, 1024], F32) for i in range(3)]
                ssq = [TB(st, "ssq%d" % i, [128, 4], F32) for i in range(4)]
                xh = [TB(st, "xh%d" % i, [128, 1024], BF16) for i in range(5)]
                xT = [st.enter_context(nc.sbuf_tensor("xT%d" % i, [128, 8, 512], BF16)) for i in range(2)]
                b_xT = [[Buf() for _ in range(4)] for _ in range(2)]
                wp = [TB(st, "wp%d" % i, [128, 8, 512], BF16) for i in range(2)]
                stg_b = [TB(st, "stgb%d" % i, [128, 512], BF16) for i in range(3)]
                stg_f = [TB(st, "stgf%d" % i, [128, 512], F32) for i in range(2)]
                sb_rot = Rot(stg_b)
                sf_rot = Rot(stg_f)
                ecount = [0]

                def ldx(tt):
                    xi, b_xi = xin[tt % 3]
                    cx.dma("sp", xi[:], x[tt * 128:(tt + 1) * 128, :], writes=[b_xi])

                def norm(tt):
                    xi, b_xi = xin[tt % 3]
                    sq, b_sq = ssq[tt % 4]
                    xhh, b_xhh = xh[tt % 5]
                    cx.op("act", lambda e: e.activation(out=xhh[:], in_=xi[:], func=AF.Square,
                                                        accum_out=sq[:, 0:1]), reads=[b_xi], writes=[b_xhh, b_sq])
                    rstd_from_ssq(sq[:, 0:1], sq[:, 1:2], sq[:, 2:3], 1024.0, [b_sq], [b_sq])
                    cx.op("dve", lambda e: e.tensor_scalar(out=xhh[:], in0=xi[:], scalar1=sq[:, 1:2], scalar2=None,
                                                           op0=ALU.mult), reads=[b_xi, b_sq], writes=[b_xhh])
                    if tt + 3 < NT:
                        ldx(tt + 3)

                def transp(tt):
                    j, t = divmod(tt, 4)
                    xhh, b_xhh = xh[tt % 5]
                    pT, b_pT = pst[tt % 2]
                    xTj = xT[j % 2]
                    bx = b_xT[j % 2]
                    for kc in range(8):
                        cx.op("pe", lambda e: e.transpose(out=pT[:, kc, :], in_=xhh[:, kc * 128:(kc + 1) * 128],
                                                          identity=ident_b), reads=[b_xhh, b_cbf], writes=[b_pT])
                    cx.op("act", lambda e: e.activation(out=xTj[:, 0:4, t * 128:(t + 1) * 128], in_=pT[:, 0:4, :],
                                                        func=AF.Copy), reads=[b_pT], writes=[bx[t]])
                    cx.op("dve", lambda e: e.tensor_copy(out=xTj[:, 4:8, t * 128:(t + 1) * 128], in_=pT[:, 4:8, :]),
                          reads=[b_pT], writes=[bx[t]])

                def ldw(j, g):
                    w, b_w = wp[(j * 7 + g) % 2]
                    cx.dma("act" if g % 2 else "sp", w[:], kcp(WP)[:, :, g * 512:(g + 1) * 512], writes=[b_w])

                def group(j, g):
                    w, b_w = wp[(j * 7 + g) % 2]
                    xTj = xT[j % 2]
                    bx = b_xT[j % 2]
                    if g < 2:
                        for h in range(4):
                            ps, b_ps = prot.next()
                            for kc in range(8):
                                cx.op("pe", lambda e: e.matmul(ps[:, :], lhsT=w[:, kc, h * 128:(h + 1) * 128],
                                                               rhs=xTj[:, kc, :], start=(kc == 0), stop=(kc == 7)),
                                      reads=[b_w] + bx, writes=[b_ps])
                            if g == 0:
                                sg_, b_sg = sb_rot.next()
                                cx.op("act", lambda e: e.activation(out=sg_[:], in_=ps[:, :], func=AF.Identity,
                                                                    bias=biascol[:, h:h + 1]),
                                      reads=[b_ps, b_biascol], writes=[b_sg])
                                cx.dma("sp", QT[h, :, j * 512:(j + 1) * 512], sg_[:], reads=[b_sg])
                            else:
                                cx.op("act", lambda e: e.activation(out=KT[:, h, j * 512:(j + 1) * 512], in_=ps[:, :],
                                                                    func=AF.Identity, bias=biascol[:, 4 + h:5 + h]),
                                      reads=[b_ps, b_biascol], writes=[b_KT[j]])
                    else:
                        for t in range(4):
                            tt = 4 * j + t
                            ps, b_ps = prot.next()
                            for kc in range(8):
                                cx.op("pe", lambda e: e.matmul(ps[:, :], lhsT=xTj[:, kc, t * 128:(t + 1) * 128],
                                                               rhs=w[:, kc, :], start=(kc == 0), stop=False),
                                      reads=[b_w, bx[t]], writes=[b_ps])
                            cx.op("pe", lambda e: e.matmul(ps[:, :], lhsT=ones_b[0:33, 0:128],
                                                           rhs=bias_hl[0:33, g * 512:(g + 1) * 512], start=False, stop=True),
                                  reads=[b_cbf, b_bias_hl], writes=[b_ps])
                            en = "act" if g in (3, 4, 6) else "dve"
                            afn = {3: AF.Silu, 4: AF.Sigmoid, 6: AF.Silu}.get(g, AF.Copy)
                            if g == 2:
                                dst = VA[:, tt, :, 0:128]
                                src = ps[:, :].rearrange("p (h v) -> p h v", h=4)
                                wb = b_VA[tt]
                            elif g == 4:
                                sg_, wb = sf_rot.next()
                                dst = sg_[:]
                                src = ps[:, :]
                            else:
                                sg_, wb = sb_rot.next()
                                dst = sg_[:]
                                src = ps[:, :]
                            if en == "act":
                                cx.op("act", lambda e: e.activation(out=dst, in_=src, func=afn),
                                      reads=[b_ps], writes=[wb])
                            else:
                                cx.op("dve", lambda e: e.tensor_copy(out=dst, in_=src), reads=[b_ps], writes=[wb])
                            if g != 2:
                                dt_ = {3: QR, 4: FR, 5: IR, 6: GR}[g]
                                cx.dma("sp", dt_[tt * 128:(tt + 1) * 128, :], dst, reads=[wb])

                for tt in range(min(3, NT)):
                    ldx(tt)
                ldw(0, 0)
                for tt in range(4):
                    norm(tt)
                    transp(tt)
                for j in range(NJ):
                    for g in range(7):
                        if g < 6:
                            ldw(j, g + 1)
                        elif j + 1 < NJ:
                            ldw(j + 1, 0)
                        if j + 1 < NJ and g < 4:
                            norm(4 * (j + 1) + g)
                        group(j, g)
                        if j + 1 < NJ and 2 <= g < 6:
                            transp(4 * (j + 1) + g - 2)
                cx.barrier()

            with ExitStack() as st:
                pss = [PB(st, "p1bs_%d" % a, [128, 2, 512], F32) for a in range(2)]
                acc = [PB(st, "p1ba_%d" % a, [128, 512], F32) for a in range(3)]
                accsb = [TB(st, "accsb%d" % a, [128, 3, 387], F32) for a in range(2)]
                qt = [TB(st, "qt%d" % i, [128, 512], BF16) for i in range(2)]
                pT = [TB(st, "pT%d" % a, [128, 2, 512], BF16) for a in range(2)]
                yast = [TB(st, "yast%d" % i, [128, 4, 512], BF16) for i in range(2)]
                sm = [TB(st, "sm%d" % i, [128, 24], F32) for i in range(4)]
                evT = [TB(st, "evT%d" % i, [128, 8, 128], F32) for i in range(2)]
                evO = [TB(st, "evO%d" % i, [128, 4, 128], F32) for i in range(2)]
                evQ = [TB(st, "evQ%d" % i, [128, 4, 128], F32) for i in range(2)]
                smrot = Rot(sm)
                tmpA = [TB(st, "tmpA%d" % i, [128, 128], F32) for i in range(2)]
                ob = [TB(st, "ob%d" % i, [128, 128], F32) for i in range(2)]
                jk2, b_jk2 = TB(st, "jk2", [128, 128], BF16)
                scale = 64 ** -0.5
                pT3 = [TB(st, "pT3_%d" % a, [128, 2, 512], BF16) for a in range(3)]
                heads = [(j, h) for j in range(NJ) for h in range(4)]
                steps = []
                for m, (j, h) in enumerate(heads):
                    for i in range(4 * j + 4):
                        steps.append((m, j, h, i))
                N = len(steps)
                started = {}

                def load_q(m):
                    j, h = heads[m]
                    q, b_q = qt[m % 2]
                    cx.dma("sp", q[:], QT[h, :, j * 512:(j + 1) * 512], writes=[b_q])

                def emit_qk(n):
                    m, j, h, i = steps[n]
                    q, b_q = qt[m % 2]
                    c0 = max(0, i - 4 * j) * 128
                    ps, b_ps = pss[n % 2]
                    for c in range(2):
                        cx.op("pe", lambda e: e.matmul(ps[:, c, c0:512], lhsT=KT[c * 64:(c + 1) * 64, h, i * 128:(i + 1) * 128],
                                                       rhs=q[c * 64:(c + 1) * 64, c0:512], start=True, stop=True),
                              reads=[b_KT[i // 4], b_q], writes=[b_ps])

                def emit_exp(n):
                    m, j, h, i = steps[n]
                    c0 = max(0, i - 4 * j) * 128
                    ps, b_ps = pss[n % 2]
                    p_, b_p = pT3[n % 3]
                    cx.op("act", lambda e: e.activation(out=p_[:, :, c0:512], in_=ps[:, :, c0:512], func=AF.Exp, scale=scale),
                          reads=[b_ps], writes=[b_p])
                    for s in range(4):
                        D = 4 * j + s - i
                        if D in (0, 1):
                            cx.op("dve", lambda e: e.tensor_tensor(out=p_[:, :, s * 128:(s + 1) * 128],
                                                                   in0=p_[:, :, s * 128:(s + 1) * 128],
                                                                   in1=EB[:, h * 2 + D, :].unsqueeze(1).to_broadcast([128, 2, 128]),
                                                                   op=ALU.mult),
                                  reads=[b_EB, b_p], writes=[b_p])

                def emit_pv(n):
                    m, j, h, i = steps[n]
                    p_, b_p = pT3[n % 3]
                    for s in range(max(0, i - 4 * j), 4):
                        for c in range(2):
                            ri = s * 2 + c
                            a_, b_a = acc[ri // 3]
                            r0 = (ri % 3) * 129
                            first = (m, ri // 3) not in started
                            started[(m, ri // 3)] = True
                            cx.op("pe", lambda e: e.matmul(a_[:, r0:r0 + 129], lhsT=p_[:, c, s * 128:(s + 1) * 128],
                                                           rhs=VA[:, i, h, :], start=first, stop=(i == 4 * j + s),
                                                           skip_group_check=True),
                                  reads=[b_p, b_VA[i]], writes=[b_a])
                    if i == 4 * j + 3:
                        emit_evac(m)

                def emit_evac(m):
                    j, h = heads[m]
                    ya, b_ya = yast[j % 2]
                    asb, b_asb = accsb[m % 2]
                    for bk in range(3):
                        nreg = 3 if bk < 2 else 2
                        cx.op("dve", lambda e: e.tensor_copy(out=asb[:, bk, 0:nreg * 129], in_=acc[bk][0][:, 0:nreg * 129]),
                              reads=[acc[bk][1]], writes=[b_asb])
                    flat = asb[:, :, :].rearrange("p a b -> p (a b)")
                    reg = flat[:, 0:8 * 129].rearrange("p (r c) -> p r c", c=129)
                    m_, b_m = smrot.next()
                    T_, b_T = evT[m % 2]
                    o4, b_o4 = evO[m % 2]
                    q4, b_q4 = evQ[m % 2]
                    b_a = b_asb
                    cx.op("dve", lambda e: e.reciprocal(out=m_[:, 0:8], in_=flat[:, 128:128 + 8 * 129:129]), reads=[b_a], writes=[b_m])
                    cx.op("dve", lambda e: e.tensor_tensor(out=T_[:, :, :], in0=reg[:, :, 0:128],
                                                           in1=m_[:, 0:8].unsqueeze(2).to_broadcast([128, 8, 128]), op=ALU.mult),
                          reads=[b_a, b_m], writes=[b_T])
                    Tv = T_[:, :, :].rearrange("p (s c) v -> p s c v", c=2)
                    cx.op("dve", lambda e: e.scalar_tensor_tensor(out=o4[:, :, :], in0=Tv[:, :, 1, :], scalar=neglam[:, 0:1],
                                                                  in1=Tv[:, :, 0, :], op0=ALU.mult, op1=ALU.add),
                          reads=[b_T, b_neglam], writes=[b_o4])
                    cx.op("dve", lambda e: e.tensor_tensor(out=q4[:, :, :], in0=o4[:, :, :], in1=o4[:, :, :], op=ALU.mult),
                          reads=[b_o4], writes=[b_q4])
                    cx.op("dve", lambda e: e.tensor_reduce(out=m_[:, 8:12], in_=q4[:, :, :], axis=AX.X, op=ALU.add),
                          reads=[b_q4, b_m], writes=[b_m])
                    rstd_from_ssq(m_[:, 8:12], m_[:, 12:16], m_[:, 16:20], 128.0, [b_m], [b_m])
                    cx.op("dve", lambda e: e.tensor_tensor(out=q4[:, :, :], in0=o4[:, :, :],
                                                           in1=m_[:, 12:16].unsqueeze(2).to_broadcast([128, 4, 128]), op=ALU.mult),
                          reads=[b_o4, b_m, b_q4], writes=[b_q4])
                    cx.op("dve", lambda e: e.tensor_tensor(out=ya[:, :, h * 128:(h + 1) * 128], in0=q4[:, :, :],
                                                           in1=sgr[:, :].unsqueeze(1).to_broadcast([128, 4, 128]), op=ALU.mult),
                          reads=[b_q4, b_sgr], writes=[b_ya])
                    if h == 3:
                        cx.dma("sp", MIX[j * 512:(j + 1) * 512, 0:512].rearrange("(s p) c -> p s c", p=128), ya[:], reads=[b_ya])

                conv = [(wi, e) for e in range(32) for wi in range(3)]
                conv.reverse()

                def emit_conv(k):
                    for _ in range(k):
                        if conv:
                            wi, e = conv.pop()
                            src = (w1r, w3r, w2r)[wi]
                            cx.dma("pool", WB[wi][e * 128:(e + 1) * 128, :], src[e * 128:(e + 1) * 128, :])

                load_q(0)
                emit_qk(0)
                for n in range(N):
                    m, j, h, i = steps[n]
                    if i == 0 and m >= 16:
                        emit_conv(2)
                    if i == 0 and m + 1 < len(heads):
                        load_q(m + 1)
                    emit_exp(n)
                    if n + 1 < N:
                        emit_qk(n + 1)
                    if n >= 1:
                        emit_pv(n - 1)
                emit_pv(N - 1)
                emit_conv(len(conv))
                cx.barrier()

        with ExitStack() as st:
            psA, b_psA = PB(st, "p1c_A", [128, 512], F32)
            psB, b_psB = PB(st, "p1c_B", [128, 512], F32)
            psC, b_psC = PB(st, "p1c_C", [128, 512], F32)
            psS, b_psS = PB(st, "p1c_S", [128, 512], F32)
            psO, b_psO = PB(st, "p1c_O", [128, 512], F32)
            psU = [PB(st, "p1c_U%d" % i, [128, 512], F32) for i in range(2)]
            psT, b_psT = PB(st, "p1c_T", [128, 8, 128], BF16)
            ld = [[TB(st, "ld%d_%d" % (a, k), [128, 512], F32 if k == 1 else BF16) for k in range(4)] for a in range(5)]
            sig, b_sig = TB(st, "sig", [128, 512], F32)
            lf_2 = [TB(st, "lf%d" % i, [128, 512], F32) for i in range(2)]
            kf_2 = [TB(st, "kf%d" % i, [128, 512], F32) for i in range(2)]
            e1, b_e1 = TB(st, "e1", [128, 512], F32)
            e1n, b_e1n = TB(st, "e1n", [128, 512], F32)
            e2, b_e2 = TB(st, "e2", [128, 512], F32)
            qs, b_qs = TB(st, "qs", [128, 512], F32)
            gg_2 = [TB(st, "gg%d" % i, [128, 512], F32) for i in range(4)]
            ebl_2 = [TB(st, "ebl%d" % i, [128, 8], F32) for i in range(3)]
            Qd_2 = [TB(st, "Qd%d" % i, [128, 512], BF16) for i in range(2)]
            Kd_2 = [TB(st, "Kd%d" % i, [128, 512], BF16) for i in range(2)]
            Kd2_2 = [TB(st, "Kd2%d" % i, [128, 512], BF16) for i in range(3)]
            QKT_2 = [TB(st, "QKT%d" % i, [128, 8, 128], BF16) for i in range(2)]
            scm_2 = [TB(st, "scm%d" % i, [128, 4, 128], BF16) for i in range(2)]
            Sf = [TB(st, "Sf%d" % h, [128, 128], F32) for h in range(4)]
            Sb = [TB(st, "Sb%d" % h, [128, 128], BF16) for h in range(4)]
            hs_, b_hs = TB(st, "hsq", [128, 16], F32)
            jk3, b_jk3 = TB(st, "jk3", [128, 128], BF16)
            yst = [TB(st, "yst%d" % i, [128, 512], BF16) for i in range(2)]
            yt_, b_yt = TB(st, "ytmp", [128, 4, 128], F32)
            lbr, b_lbr = TB(st, "lbr", [128, 512], F32)
            omlr, b_omlr = TB(st, "omlr", [128, 512], F32)
            rngr, b_rngr = TB(st, "rngr", [128, 512], F32)
            cx.dma("sp", rngr[:], rng_rep, writes=[b_rngr])
            cx.dma("sp", e1[:], rlb_rep[:, 0:512], writes=[b_e1])
            cx.dma("sp", e2[:], rlb_rep[:, 512:1024], writes=[b_e2])
            cx.op("dve", lambda e: e.tensor_tensor(out=lbr[:], in0=e1[:], in1=e2[:], op=ALU.subtract),
                  reads=[b_e1, b_e2], writes=[b_lbr])
            cx.op("act", lambda e: e.activation(out=lbr[:], in_=lbr[:], func=AF.Sigmoid), reads=[b_lbr], writes=[b_lbr])
            cx.op("dve", lambda e: e.tensor_scalar(out=omlr[:], in0=lbr[:], scalar1=-1.0, scalar2=1.0,
                                                   op0=ALU.mult, op1=ALU.add), reads=[b_lbr], writes=[b_omlr])
            for h in range(4):
                cx.op("pool", lambda e: e.memset(Sf[h][0][:], 0.0), writes=[Sf[h][1]])
                cx.op("pool", lambda e: e.memset(Sb[h][0][:], 0.0), writes=[Sb[h][1]])
            def ld1c(tt):
                (qr_, b_qr), (fr_, b_fr), (ir_, b_ir), (gr_, b_gr) = ld[tt % 5]
                rows = slice(tt * 128, (tt + 1) * 128)
                cx.dma("sp", qr_[:], QR[rows, :], writes=[b_qr])
                cx.dma("sp", fr_[:], FR[rows, :], writes=[b_fr])
                cx.dma("sp", ir_[:], IR[rows, :], writes=[b_ir])
                cx.dma("sp", gr_[:], GR[rows, :], writes=[b_gr])

            def stA0_1c(tt):
                (qr_, b_qr), (fr_, b_fr), (ir_, b_ir), (gr_, b_gr) = ld[tt % 5]
                gg, b_gg = gg_2[tt % 4]
                lf, b_lf = lf_2[tt % 2]
                kf, b_kf = kf_2[tt % 2]
                if tt + 1 < NT:
                    ld1c(tt + 1)
                cx.op("dve", lambda e: e.tensor_tensor(out=sig[:], in0=fr_[:], in1=omlr[:], op=ALU.mult),
                      reads=[b_fr, b_omlr], writes=[b_sig])
                cx.op("dve", lambda e: e.tensor_tensor(out=sig[:], in0=sig[:], in1=lbr[:], op=ALU.add),
                      reads=[b_sig, b_lbr], writes=[b_sig])
                cx.op("act", lambda e: e.activation(out=lf[:], in_=sig[:], func=AF.Ln), reads=[b_sig], writes=[b_lf])
                cx.op("act", lambda e: e.activation(out=kf[:], in_=sig[:], func=AF.Identity, scale=-1.0, bias=1.0),
                      reads=[b_sig], writes=[b_kf])
                cx.op("dve", lambda e: e.tensor_tensor(out=gg[:], in0=gr_[:], in1=rngr[:], op=ALU.mult),
                      reads=[b_gr, b_rngr], writes=[b_gg])

            def stA1c(tt):
                (qr_, b_qr), (fr_, b_fr), (ir_, b_ir), (gr_, b_gr) = ld[tt % 5]
                lf, b_lf = lf_2[tt % 2]
                kf, b_kf = kf_2[tt % 2]
                ebl, b_ebl = ebl_2[tt % 3]
                Kd2, b_Kd2 = Kd2_2[tt % 3]
                Qd, b_Qd = Qd_2[tt % 2]
                Kd, b_Kd = Kd_2[tt % 2]
                cx.op("pe", lambda e: e.matmul(psA[:, :], lhsT=L1, rhs=lf[:], start=True, stop=True),
                      reads=[b_cst, b_lf], writes=[b_psA])
                cx.op("pe", lambda e: e.matmul(psB[:, :], lhsT=L2, rhs=lf[:], start=True, stop=True),
                      reads=[b_cst, b_lf], writes=[b_psB])
                for h in range(4):
                    cx.op("pe", lambda e: e.matmul(psC[:, 2 * h:2 * h + 2], lhsT=lf[:, h * 128:(h + 1) * 128], rhs=sel2,
                                                   start=True, stop=True), reads=[b_cst, b_lf], writes=[b_psC])
                cx.op("act", lambda e: e.activation(out=e1[:], in_=psA[:, :], func=AF.Exp), reads=[b_psA], writes=[b_e1])
                cx.op("act", lambda e: e.activation(out=e1n[:], in_=psA[:, :], func=AF.Exp, scale=-1.0),
                      reads=[b_psA], writes=[b_e1n])
                cx.op("act", lambda e: e.activation(out=e2[:], in_=psB[:, :], func=AF.Exp), reads=[b_psB], writes=[b_e2])
                cx.op("act", lambda e: e.activation(out=ebl[:], in_=psC[:, 0:8], func=AF.Exp), reads=[b_psC], writes=[b_ebl])
                cx.op("dve", lambda e: e.tensor_tensor(out=Qd[:], in0=qr_[:], in1=e1[:], op=ALU.mult),
                      reads=[b_qr, b_e1], writes=[b_Qd])
                cx.op("dve", lambda e: e.tensor_tensor(out=Kd[:], in0=kf[:], in1=e1n[:], op=ALU.mult),
                      reads=[b_kf, b_e1n], writes=[b_Kd])
                cx.op("dve", lambda e: e.tensor_tensor(out=Kd2[:], in0=kf[:], in1=e2[:], op=ALU.mult),
                      reads=[b_kf, b_e2], writes=[b_Kd2])

            def stA2_1c(tt):
                Qd, b_Qd = Qd_2[tt % 2]
                Kd, b_Kd = Kd_2[tt % 2]
                QKT, b_QKT = QKT_2[tt % 2]
                scm, b_scm = scm_2[tt % 2]
                for h in range(4):
                    cx.op("pe", lambda e: e.transpose(out=psT[:, h, :], in_=Qd[:, h * 128:(h + 1) * 128], identity=ident_b),
                          reads=[b_Qd, b_cbf], writes=[b_psT])
                for h in range(4):
                    cx.op("pe", lambda e: e.transpose(out=psT[:, 4 + h, :], in_=Kd[:, h * 128:(h + 1) * 128], identity=ident_b),
                          reads=[b_Kd, b_cbf], writes=[b_psT])
                cx.op("act", lambda e: e.activation(out=QKT[:, 0:4, :], in_=psT[:, 0:4, :], func=AF.Copy),
                      reads=[b_psT], writes=[b_QKT])
                cx.op("dve", lambda e: e.tensor_copy(out=QKT[:, 4:8, :], in_=psT[:, 4:8, :]), reads=[b_psT], writes=[b_QKT])
                for h in range(4):
                    cx.op("pe", lambda e: e.matmul(psS[:, h * 128:(h + 1) * 128], lhsT=QKT[:, 4 + h, :], rhs=QKT[:, h, :],
                                                   start=True, stop=True), reads=[b_QKT], writes=[b_psS])
                cx.op("dve", lambda e: e.tensor_tensor(out=scm[:, :, :], in0=psS[:, :].rearrange("p (h t) -> p h t", h=4),
                                                       in1=MT.unsqueeze(1).to_broadcast([128, 4, 128]), op=ALU.mult),
                      reads=[b_psS, b_cst], writes=[b_scm])

            def stB1c(tt):
                (qr_, b_qr), (fr_, b_fr), (ir_, b_ir), (gr_, b_gr) = ld[tt % 5]
                gg, b_gg = gg_2[tt % 4]
                ebl, b_ebl = ebl_2[tt % 3]
                Kd2, b_Kd2 = Kd2_2[tt % 3]
                QKT, b_QKT = QKT_2[tt % 2]
                scm, b_scm = scm_2[tt % 2]
                rows = slice(tt * 128, (tt + 1) * 128)
                for h in range(4):
                    hv = slice(h * 128, (h + 1) * 128)
                    cx.op("act", lambda e: e.activation(out=Sb[h][0][:], in_=Sf[h][0][:], func=AF.Copy, scale=ebl[:, 2 * h + 1:2 * h + 2]),
                          reads=[Sf[h][1], b_ebl], writes=[Sb[h][1]])
                    cx.op("pe", lambda e: e.matmul(psO[:, hv], lhsT=QKT[:, h, :], rhs=Sb[h][0][:], start=True, stop=False),
                          reads=[b_QKT, Sb[h][1]], writes=[b_psO])
                    cx.op("pe", lambda e: e.matmul(psO[:, hv], lhsT=scm[:, h, :], rhs=ir_[:, hv], start=False, stop=True),
                          reads=[b_scm, b_ir], writes=[b_psO])
                    pu, b_pu = psU[h % 2]
                    cx.op("pe", lambda e: e.matmul(pu[:, 0:128], lhsT=Kd2[:, hv], rhs=ir_[:, hv], start=True, stop=True),
                          reads=[b_Kd2, b_ir], writes=[b_pu])
                    cx.op("dve", lambda e: e.scalar_tensor_tensor(out=Sf[h][0][:], in0=Sf[h][0][:], scalar=ebl[:, 2 * h:2 * h + 1],
                                                                  in1=pu[:, 0:128], op0=ALU.mult, op1=ALU.add),
                          reads=[b_ebl, b_pu, Sf[h][1]], writes=[Sf[h][1]])
                for h in range(4):
                    cx.op("act", lambda e: e.activation(out=jk3[:], in_=psO[:, h * 128:(h + 1) * 128], func=AF.Square,
                                                        accum_out=hs_[:, h:h + 1]), reads=[b_psO], writes=[b_jk3, b_hs])
                rstd_from_ssq(hs_[:, 0:4], hs_[:, 4:8], hs_[:, 8:12], 128.0, [b_hs], [b_hs], via="act")
                y_, b_y = yst[tt % 2]
                cx.op("dve", lambda e: e.tensor_tensor(out=yt_[:, :, :], in0=psO[:, :].rearrange("p (h v) -> p h v", h=4),
                                                       in1=hs_[:, 4:8].unsqueeze(2).to_broadcast([128, 4, 128]), op=ALU.mult),
                      reads=[b_psO, b_hs], writes=[b_yt])
                cx.op("dve", lambda e: e.tensor_tensor(out=y_[:, :], in0=yt_[:, :, :].rearrange("p h v -> p (h v)"), in1=gg[:, :],
                                                       op=ALU.mult), reads=[b_yt, b_gg], writes=[b_y])
                cx.dma("sp", MIX[rows, 512:1024], y_[:], reads=[b_y])

            ld1c(0)
            for it in range(-3, NT):
                fns = []
                if 0 <= it + 3 < NT:
                    fns.append(lambda: stA0_1c(it + 3))
                if 0 <= it + 2 < NT:
                    fns.append(lambda: stA1c(it + 2))
                if 0 <= it + 1 < NT:
                    fns.append(lambda: stA2_1c(it + 1))
                if it >= 0:
                    fns.append(lambda: stB1c(it))
                cx.zip_emit(fns)
            cx.barrier()

        with ExitStack() as st:
            psX = [PB(st, "p1d_X%d" % i, [128, 512], F32) for i in range(2)]
            psT, b_psT = PB(st, "p1d_T", [128, 8, 128], BF16)
            psH = [PB(st, "p1d_H%d" % i, [128, 512], F32) for i in range(2)]
            psL, b_psL = PB(st, "p1d_L", [128, 512], F32)
            psR, b_psR = PB(st, "p1d_R", [128, 512], F32)
            wop, b_wop = TB(st, "wop", [128, 8, 1024], BF16)
            wrt, b_wrt = TB(st, "wrt", [128, 8, 36], F32)
            brr, b_brr = TB(st, "brr", [128, 36], F32)
            mix = [TB(st, "mix%d" % i, [128, 1024], BF16) for i in range(3)]
            mixT, b_mixT = TB(st, "mixT", [128, 8, 128], BF16)
            xin = [TB(st, "xin1d%d" % i, [128, 1024], F32) for i in range(3)]
            x1 = [TB(st, "x1_%d" % i, [128, 1024], F32) for i in range(2)]
            h2_2 = [TB(st, "h2_%d" % i, [128, 1024], F32) for i in range(2)]
            h2b = [TB(st, "h2b%d" % i, [128, 1024], BF16) for i in range(2)]
            h2T, b_h2T = TB(st, "h2T", [128, 8, 128], F32)
            jk4, b_jk4 = TB(st, "jk4", [128, 1024], BF16)
            sq, b_sq = TB(st, "sq1d", [128, 4], F32)
            lg_2 = [TB(st, "lg%d" % i, [128, 36], F32) for i in range(2)]
            lgT, b_lgT = TB(st, "lgT", [36, 128], F32)
            rt, b_rt = TB(st, "rt", [128, 16], F32)
            gsel, b_gsel = TB(st, "gsel", [128, 4], F32)
            ge, b_ge = TB(st, "ge", [128, 4], F32)
            emask, b_emask = TB(st, "emask", [128, 32], F32)
            elm, b_elm = TB(st, "elm", [128, 32], F32)
            t32_, b_t32 = TB(st, "t32r", [128, 32], F32)
            OH, _ = TB(st, "OH", [128, NT * 2, 32], F32)
            b_OHt = [Buf() for _ in range(NT)]
            cntb2 = [TB(st, "cntb%d" % i, [128, 32], BF16) for i in range(2)]
            cnt2 = [TB(st, "cnt32_%d" % i, [128, 32], F32) for i in range(2)]
            elmb, b_elmb = TB(st, "elmb", [128, 32], F32)
            cnts, b_cnts = TB(st, "cnts", [128, 32], F32)
            cntsb, b_cntsb = TB(st, "cntsb", [128, 32], BF16)
            RK, b_RK = TB(st, "RK", [128, NT * 2], F32)
            tot, b_tot = TB(st, "tot", [128, 32], F32)
            pad, b_pad = TB(st, "pad", [128, 32], F32)
            pend, b_pend = TB(st, "pend", [128, 32], F32)
            pstart, b_pstart = TB(st, "pstart", [128, 32], F32)
            dacc, b_dacc = TB(st, "dacc", [128, NT * 2], F32)
            be, b_be = TB(st, "be", [128, NB], F32)
            a2rep, b_a2rep = TB(st, "a2rep1d", [128, 1024], F32)
            sh2rep, b_sh2rep = TB(st, "sh2rep1d", [128, 1024], F32)
            cx.dma("sp", a2rep[:], REPS[0], writes=[b_a2rep])
            cx.dma("sp", sh2rep[:], REPS[1], writes=[b_sh2rep])
            cx.dma("sp", wop[:], kcp(WOP), writes=[b_wop])
            cx.dma("sp", wrt[:], wr, writes=[b_wrt])
            cx.dma("sp", brr[:], br_rep, writes=[b_brr])
            cx.op("pool", lambda e: e.memset(cnts[:], 0.0), writes=[b_cnts])
            cx.op("pool", lambda e: e.memset(cntsb[:], 0.0), writes=[b_cntsb])
            def ld1d(tt):
                rows = slice(tt * 128, (tt + 1) * 128)
                mx, b_mx = mix[tt % 3]
                xi, b_xi = xin[tt % 3]
                cx.dma("sp", mx[:], MIX[rows, :], writes=[b_mx])
                cx.dma("sp", xi[:], x[rows, :], writes=[b_xi])

            def stC1d(tt):
                rows = slice(tt * 128, (tt + 1) * 128)
                mx, b_mx = mix[tt % 3]
                xi, b_xi = xin[tt % 3]
                h2, b_h2 = h2_2[tt % 2]
                if tt + 1 < NT:
                    ld1d(tt + 1)
                x1_, b_x1 = x1[tt % 2]
                hb, b_hb = h2b[tt % 2]
                for kc in range(8):
                    cx.op("pe", lambda e: e.transpose(out=psT[:, kc, :], in_=mx[:, kc * 128:(kc + 1) * 128], identity=ident_b),
                          reads=[b_mx, b_cbf], writes=[b_psT])
                cx.op("act", lambda e: e.activation(out=mixT[:, 0:4, :], in_=psT[:, 0:4, :], func=AF.Copy),
                      reads=[b_psT], writes=[b_mixT])
                cx.op("dve", lambda e: e.tensor_copy(out=mixT[:, 4:8, :], in_=psT[:, 4:8, :]), reads=[b_psT], writes=[b_mixT])
                for half in range(2):
                    px, b_px = psX[half]
                    for kc in range(8):
                        cx.op("pe", lambda e: e.matmul(px[:, :], lhsT=mixT[:, kc, :], rhs=wop[:, kc, half * 512:(half + 1) * 512],
                                                       start=(kc == 0), stop=(kc == 7)), reads=[b_mixT, b_wop], writes=[b_px])
                    cx.op("dve", lambda e: e.tensor_tensor(out=x1_[:, half * 512:(half + 1) * 512], in0=px[:, :],
                                                           in1=xi[:, half * 512:(half + 1) * 512], op=ALU.add),
                          reads=[b_px, b_xi], writes=[b_x1])
                cx.dma("sp", X1[rows, :], x1_[:], reads=[b_x1])
                cx.op("act", lambda e: e.activation(out=jk4[:], in_=x1_[:], func=AF.Square, accum_out=sq[:, 0:1]),
                      reads=[b_x1], writes=[b_jk4, b_sq])
                rstd_from_ssq(sq[:, 0:1], sq[:, 1:2], sq[:, 2:3], 1024.0, [b_sq], [b_sq], via="act")
                cx.op("dve", lambda e: e.scalar_tensor_tensor(out=h2[:], in0=x1_[:], scalar=sq[:, 1:2], in1=a2rep[:],
                                                              op0=ALU.mult, op1=ALU.mult),
                      reads=[b_x1, b_sq, b_a2rep], writes=[b_h2])
                cx.op("dve", lambda e: e.tensor_tensor(out=h2[:], in0=h2[:], in1=sh2rep[:], op=ALU.add),
                      reads=[b_h2, b_sh2rep], writes=[b_h2])
                cx.op("act", lambda e: e.activation(out=hb[:], in_=h2[:], func=AF.Copy), reads=[b_h2], writes=[b_hb])
                cx.dma("sp", H2[rows, :], hb[:], reads=[b_hb])

            def stC2_1d(tt):
                h2, b_h2 = h2_2[tt % 2]
                lg, b_lg = lg_2[tt % 2]
                for kc in range(8):
                    ph, b_ph = psH[kc // 4]
                    cx.op("pe", lambda e: e.transpose(out=ph[:, (kc % 4) * 128:(kc % 4 + 1) * 128],
                                                      in_=h2[:, kc * 128:(kc + 1) * 128], identity=ident_f),
                          reads=[b_h2, b_cst], writes=[b_ph])
                cx.op("act", lambda e: e.activation(out=h2T[:, 0:4, :], in_=psH[0][0][:, :].rearrange("p (a b) -> p a b", a=4),
                                                    func=AF.Copy), reads=[psH[0][1]], writes=[b_h2T])
                cx.op("dve", lambda e: e.tensor_copy(out=h2T[:, 4:8, :], in_=psH[1][0][:, :].rearrange("p (a b) -> p a b", a=4)),
                      reads=[psH[1][1]], writes=[b_h2T])
                for kc in range(8):
                    cx.op("pe", lambda e: e.matmul(psL[0:36, 0:128], lhsT=wrt[:, kc, :], rhs=h2T[:, kc, :], start=(kc == 0), stop=(kc == 7)),
                          reads=[b_h2T, b_wrt], writes=[b_psL])
                cx.op("act", lambda e: e.activation(out=lgT[0:36, :], in_=psL[0:36, 0:128], func=AF.Copy), reads=[b_psL], writes=[b_lgT])
                cx.op("pe", lambda e: e.transpose(out=psL[:, 128:164], in_=lgT[0:36, :], identity=ident_f[0:36, 0:36]),
                      reads=[b_lgT, b_cst], writes=[b_psL])
                cx.op("dve", lambda e: e.tensor_tensor(out=lg[:], in0=psL[:, 128:164], in1=brr[:], op=ALU.add),
                      reads=[b_psL, b_brr], writes=[b_lg])
            D_ = lambda fn, r, w: cx.op("dve", fn, reads=r, writes=w)

            def stD1d(tt):
                lg, b_lg = lg_2[tt % 2]
                b_OH = b_OHt[tt]
                D_(lambda e: e.tensor_reduce(out=rt[:, 0:1], in_=lg[:, 0:4], axis=AX.X, op=ALU.max), [b_lg], [b_rt])
                D_(lambda e: e.tensor_scalar(out=rt[:, 1:2], in0=rt[:, 0:1], scalar1=-1.0, scalar2=None, op0=ALU.mult), [b_rt], [b_rt])
                cx.op("act", lambda e: e.activation(out=ge[:], in_=lg[:, 0:4], func=AF.Exp, bias=rt[:, 1:2], accum_out=rt[:, 2:3]),
                      reads=[b_lg, b_rt], writes=[b_ge, b_rt])
                D_(lambda e: e.reciprocal(out=rt[:, 3:4], in_=rt[:, 2:3]), [b_rt], [b_rt])
                D_(lambda e: e.tensor_scalar(out=gsel[:], in0=lg[:, 0:4], scalar1=rt[:, 0:1], scalar2=None, op0=ALU.is_equal),
                   [b_lg, b_rt], [b_gsel])
                D_(lambda e: e.tensor_copy(out=emask[:, :].rearrange("p (g e) -> p g e", g=4),
                                           in_=gsel[:, :].unsqueeze(2).to_broadcast([128, 4, 8])), [b_gsel], [b_emask])
                D_(lambda e: e.tensor_tensor(out=elm[:], in0=lg[:, 4:36], in1=emask[:], op=ALU.mult), [b_lg, b_emask], [b_elm])
                D_(lambda e: e.tensor_scalar(out=t32_[:], in0=emask[:], scalar1=-1.0, scalar2=1e9, op0=ALU.add, op1=ALU.mult),
                   [b_emask], [b_t32])
                D_(lambda e: e.tensor_tensor(out=elm[:], in0=elm[:], in1=t32_[:], op=ALU.add), [b_elm, b_t32], [b_elm])
                oh1 = OH[:, 2 * tt, :]
                oh2 = OH[:, 2 * tt + 1, :]
                D_(lambda e: e.tensor_reduce(out=rt[:, 4:5], in_=elm[:], axis=AX.X, op=ALU.max), [b_elm], [b_rt])
                D_(lambda e: e.tensor_scalar(out=oh1, in0=elm[:], scalar1=rt[:, 4:5], scalar2=None, op0=ALU.is_equal),
                   [b_elm, b_rt], [b_OH])
                D_(lambda e: e.scalar_tensor_tensor(out=elm[:], in0=oh1, scalar=-1e9, in1=elm[:], op0=ALU.mult, op1=ALU.add),
                   [b_OH, b_elm], [b_elm])
                D_(lambda e: e.tensor_reduce(out=rt[:, 5:6], in_=elm[:], axis=AX.X, op=ALU.max), [b_elm], [b_rt])
                D_(lambda e: e.tensor_scalar(out=oh2, in0=elm[:], scalar1=rt[:, 5:6], scalar2=None, op0=ALU.is_equal),
                   [b_elm, b_rt], [b_OH])
                D_(lambda e: e.tensor_scalar(out=rt[:, 6:7], in0=rt[:, 4:5], scalar1=-1.0, scalar2=None, op0=ALU.mult), [b_rt], [b_rt])
                cx.op("act", lambda e: e.activation(out=rt[:, 7:8], in_=rt[:, 5:6], func=AF.Exp, bias=rt[:, 6:7]),
                      reads=[b_rt], writes=[b_rt])
                D_(lambda e: e.tensor_scalar(out=rt[:, 8:9], in0=rt[:, 7:8], scalar1=1.0, scalar2=None, op0=ALU.add), [b_rt], [b_rt])
                D_(lambda e: e.reciprocal(out=rt[:, 9:10], in_=rt[:, 8:9]), [b_rt], [b_rt])
                D_(lambda e: e.tensor_tensor(out=WTS[:, tt, 0:1], in0=rt[:, 9:10], in1=rt[:, 3:4], op=ALU.mult), [b_rt], [b_WTS])
                D_(lambda e: e.tensor_tensor(out=WTS[:, tt, 1:2], in0=WTS[:, tt, 0:1], in1=rt[:, 7:8], op=ALU.mult),
                   [b_rt, b_WTS], [b_WTS])
                c32, b_c32 = cnt2[tt % 2]
                cb_, b_cb = cntb2[tt % 2]
                D_(lambda e: e.tensor_tensor(out=c32[:], in0=oh1, in1=oh2, op=ALU.add), [b_OH], [b_c32])
                D_(lambda e: e.tensor_copy(out=cb_[:], in_=c32[:]), [b_c32], [b_cb])

            def stDb1d(tt):
                b_OH = b_OHt[tt]
                c32, b_c32 = cnt2[tt % 2]
                cb_, b_cb = cntb2[tt % 2]
                cx.op("pe", lambda e: e.matmul(psR[:, 0:32], lhsT=US_b, rhs=cb_[:], start=True, stop=False),
                      reads=[b_cbf, b_cb], writes=[b_psR])
                cx.op("pe", lambda e: e.matmul(psR[:, 0:32], lhsT=ones_b, rhs=cntsb[:], start=False, stop=True),
                      reads=[b_cbf, b_cntsb], writes=[b_psR])
                for k in range(2):
                    ohk = OH[:, 2 * tt + k, :]
                    D_(lambda e: e.tensor_tensor(out=elmb[:], in0=psR[:, 0:32], in1=ohk, op=ALU.mult), [b_psR, b_OH, b_elmb], [b_elmb])
                    D_(lambda e: e.tensor_reduce(out=RK[:, 2 * tt + k:2 * tt + k + 1], in_=elmb[:], axis=AX.X, op=ALU.add),
                       [b_elmb], [b_RK])
                D_(lambda e: e.tensor_tensor(out=cnts[:], in0=cnts[:], in1=c32[:], op=ALU.add), [b_cnts, b_c32], [b_cnts])
                D_(lambda e: e.tensor_copy(out=cntsb[:], in_=cnts[:]), [b_cnts], [b_cntsb])
            ld1d(0)
            stC1d(0)
            if NT > 1:
                cx.zip_emit([lambda: stC1d(1), lambda: stC2_1d(0)])
            else:
                stC2_1d(0)
            for tt in range(NT):
                fns = []
                if tt + 2 < NT:
                    fns.append(lambda: stC1d(tt + 2))
                if tt + 1 < NT:
                    fns.append(lambda: stC2_1d(tt + 1))
                fns.append(lambda: stD1d(tt))
                if tt >= 1:
                    fns.append(lambda: stDb1d(tt - 1))
                cx.zip_emit(fns)
            stDb1d(NT - 1)
            cx.op("pe", lambda e: e.matmul(psR[:, 0:32], lhsT=ones_b, rhs=cntsb[:], start=True, stop=True),
                  reads=[b_cbf, b_cntsb], writes=[b_psR])
            D_(lambda e: e.tensor_copy(out=tot[:], in_=psR[:, 0:32]), [b_psR], [b_tot])
            cx.op("pool", lambda e: e.memset(pad[:], 0.0), writes=[b_pad])
            for m_ in range((2 * S) // BLK):
                D_(lambda e: e.scalar_tensor_tensor(out=pad[:], in0=tot[:], scalar=float(m_ * BLK), in1=pad[:],
                                                    op0=ALU.is_gt, op1=ALU.add), [b_tot, b_pad], [b_pad])
            D_(lambda e: e.tensor_scalar(out=pad[:], in0=pad[:], scalar1=float(BLK), scalar2=None, op0=ALU.mult), [b_pad], [b_pad])
            D_(lambda e: e.tensor_copy(out=pend[:, 0:1], in_=pad[:, 0:1]), [b_pad], [b_pend])
            for e_ in range(1, 32):
                D_(lambda e: e.tensor_tensor(out=pend[:, e_:e_ + 1], in0=pend[:, e_ - 1:e_], in1=pad[:, e_:e_ + 1], op=ALU.add),
                   [b_pad, b_pend], [b_pend])
            D_(lambda e: e.tensor_tensor(out=pstart[:], in0=pend[:], in1=pad[:], op=ALU.subtract), [b_pend, b_pad], [b_pstart])
            D_(lambda e: e.tensor_copy(out=dacc[:], in_=RK[:]), [b_RK], [b_dacc])
            for e_ in range(32):
                D_(lambda e: e.scalar_tensor_tensor(out=dacc[:], in0=OH[:, :, e_], scalar=pstart[:, e_:e_ + 1], in1=dacc[:],
                                                    op0=ALU.mult, op1=ALU.add), b_OHt + [b_pstart, b_dacc], [b_dacc])
            D_(lambda e: e.tensor_copy(out=DESTi[:].rearrange("p t k -> p (t k)"), in_=dacc[:]), [b_dacc], [b_DESTi])
            cx.op("pool", lambda e: e.memset(be[:], 0.0), writes=[b_be])
            for e_ in range(31):
                D_(lambda e: e.scalar_tensor_tensor(out=be[:], in0=bpos, scalar=pend[:, e_:e_ + 1], in1=be[:],
                                                    op0=ALU.is_ge, op1=ALU.add), [b_cst, b_pend, b_be], [b_be])
            same, b_same = TB(st, "same", [128, NB], F32)
            cx.op("pool", lambda e: e.memset(same[:], 0.0), writes=[b_same])
            D_(lambda e: e.tensor_tensor(out=same[:, 2:NB], in0=be[:, 2:NB], in1=be[:, 0:NB - 2], op=ALU.is_equal),
               [b_be, b_same], [b_same])
            D_(lambda e: e.tensor_scalar(out=be[:], in0=be[:], scalar1=128.0, scalar2=piota, op0=ALU.mult, op1=ALU.add),
               [b_be, b_cst], [b_be])
            D_(lambda e: e.scalar_tensor_tensor(out=be[:], in0=same[:], scalar=1048576.0, in1=be[:], op0=ALU.mult, op1=ALU.add),
               [b_same, b_be], [b_be])
            D_(lambda e: e.tensor_copy(out=widx[:], in_=be[:]), [b_be], [b_widx])
            if debug:
                cx.dma("sp", DBG[:, 0:2 * NT], dacc[:], reads=[b_dacc])
                cx.dma("sp", DBG[:, 512:512 + NB], be[:], reads=[b_be])
                cx.dma("sp", DBG[:, 1024:1024 + 2 * NT], WTS[:].rearrange("p t k -> p (t k)"), reads=[b_WTS])
                cx.dma("sp", DBG[:, 2048:2048 + 32], pend[:], reads=[b_pend])
            cx.barrier()

        with ExitStack() as st:
            hb = [TB(st, "hb1e%d" % i, [128, 1024], BF16) for i in range(4)]
            for tt in range(NT):
                t_, b_t = hb[tt % 4]
                cx.dma("sp", t_[:], H2[tt * 128:(tt + 1) * 128, :], writes=[b_t])
                for k in range(2):
                    cx.idma(out=HS, out_off=DESTi[:, tt, k:k + 1], in_=t_[:], in_off=None,
                            reads=[b_t, b_DESTi])
            cx.barrier()

        with ExitStack() as st:
            psT, b_psT = PB(st, "p2_T", [128, 8, 128], BF16)
            psA = [PB(st, "p2_A%d" % i, [128, 512], F32) for i in range(2)]
            psB = [PB(st, "p2_B%d" % i, [128, 512], F32) for i in range(2)]
            psY = [PB(st, "p2_Y%d" % i, [128, 512], F32) for i in range(2)]
            psT2, b_psT2 = PB(st, "p2_T2", [128, 4, 128], BF16)
            W1 = [TB(st, "W1_%d" % i, [128, 8, 512], BF16) for i in range(2)]
            W3 = [TB(st, "W3_%d" % i, [128, 8, 512], BF16) for i in range(2)]
            W2 = [TB(st, "W2_%d" % i, [128, 4, 1024], BF16) for i in range(2)]
            hs = [TB(st, "hs%d" % i, [128, 1024], BF16) for i in range(4)]
            yo = [TB(st, "yo%d" % i, [128, 1024], BF16) for i in range(2)]

            def load_w(b, which=(0, 1, 2)):
                for wi, (Wl, src) in enumerate(((W1, WB[0]), (W3, WB[1]), (W2, WB[2]))):
                    if wi not in which:
                        continue
                    w_, b_w = Wl[b % 2]
                    cx.idma(out=w_[:].rearrange("p a b -> p (a b)"), out_off=None, in_=src, in_off=widx[:, b:b + 1],
                            reads=[b_widx], writes=[b_w], bounds=wbound)

            g2rep, b_g2rep = TB(st, "g2rep2", [128, 1024], F32)
            cx.dma("sp", g2rep[:], REPS[2], writes=[b_g2rep])
            hT2 = [TB(st, "hT2_%d" % i, [128, 8, 128], BF16) for i in range(2)]
            sa2 = [TB(st, "sa2_%d" % i, [128, 512], F32) for i in range(2)]
            hid2 = [TB(st, "hid2_%d" % i, [128, 512], BF16) for i in range(2)]
            hidT2 = [TB(st, "hidT2_%d" % i, [128, 4, 128], BF16) for i in range(2)]
            NSUB = NB * (BLK // 128)
            SPB = BLK // 128

            def wts(n):
                b = n // SPB
                return W1[b % 2], W3[b % 2], W2[b % 2]

            def ldA(n):
                r0 = n * 128
                h_, b_h = hs[n % 4]
                cx.dma("sp", h_[:], HS[r0:r0 + 128, :], writes=[b_h])

            def stA(n):
                h_, b_h = hs[n % 4]
                t_, b_t = hT2[n % 2]
                for kc in range(8):
                    cx.op("pe", lambda e: e.transpose(out=psT[:, kc, :], in_=h_[:, kc * 128:(kc + 1) * 128], identity=ident_b),
                          reads=[b_h, b_cbf], writes=[b_psT])
                cx.op("act", lambda e: e.activation(out=t_[:, 0:4, :], in_=psT[:, 0:4, :], func=AF.Copy),
                      reads=[b_psT], writes=[b_t])
                cx.op("dve", lambda e: e.tensor_copy(out=t_[:, 4:8, :], in_=psT[:, 4:8, :]), reads=[b_psT], writes=[b_t])

            def stB(n):
                (w1_, b_w1), (w3_, b_w3), _ = wts(n)
                t_, b_t = hT2[n % 2]
                pa, b_pa = psA[n % 2]
                pb, b_pb = psB[n % 2]
                sa_, b_sa = sa2[n % 2]
                hd, b_hd = hid2[n % 2]
                for kc in range(8):
                    cx.op("pe", lambda e: e.matmul(pa[:, :], lhsT=t_[:, kc, :], rhs=w1_[:, kc, :], start=(kc == 0), stop=(kc == 7)),
                          reads=[b_t, b_w1], writes=[b_pa])
                for kc in range(8):
                    cx.op("pe", lambda e: e.matmul(pb[:, :], lhsT=t_[:, kc, :], rhs=w3_[:, kc, :], start=(kc == 0), stop=(kc == 7)),
                          reads=[b_t, b_w3], writes=[b_pb])
                cx.op("act", lambda e: e.activation(out=sa_[:], in_=pa[:, :], func=AF.Silu), reads=[b_pa], writes=[b_sa])
                cx.op("dve", lambda e: e.tensor_tensor(out=hd[:], in0=pb[:, :], in1=sa_[:], op=ALU.mult),
                      reads=[b_pb, b_sa], writes=[b_hd])

            def stC(n):
                hd, b_hd = hid2[n % 2]
                ht, b_ht = hidT2[n % 2]
                for fc in range(4):
                    cx.op("pe", lambda e: e.transpose(out=psT2[:, fc, :], in_=hd[:, fc * 128:(fc + 1) * 128], identity=ident_b),
                          reads=[b_hd, b_cbf], writes=[b_psT2])
                cx.op("act", lambda e: e.activation(out=ht[:, 0:2, :], in_=psT2[:, 0:2, :], func=AF.Copy),
                      reads=[b_psT2], writes=[b_ht])
                cx.op("dve", lambda e: e.tensor_copy(out=ht[:, 2:4, :], in_=psT2[:, 2:4, :]), reads=[b_psT2], writes=[b_ht])

            def stD(n):
                _, _, (w2_, b_w2) = wts(n)
                r0 = n * 128
                ht, b_ht = hidT2[n % 2]
                y_, b_y = yo[n % 2]
                for half in range(2):
                    py, b_py = psY[half]
                    for fc in range(4):
                        cx.op("pe", lambda e: e.matmul(py[:, :], lhsT=ht[:, fc, :], rhs=w2_[:, fc, half * 512:(half + 1) * 512],
                                                       start=(fc == 0), stop=(fc == 3)), reads=[b_ht, b_w2], writes=[b_py])
                    cx.op("dve", lambda e: e.tensor_tensor(out=y_[:, half * 512:(half + 1) * 512], in0=py[:, :],
                                                           in1=g2rep[:, half * 512:(half + 1) * 512], op=ALU.mult),
                          reads=[b_py, b_g2rep], writes=[b_y])
                cx.dma("sp", YS[r0:r0 + 128, :], y_[:], reads=[b_y])

            load_w(0)
            if NB > 1:
                load_w(1)
            ldA(0)
            ldA(1)
            ldA(2)
            stA(0)
            for n in range(NSUB + 1):
                if n + 3 < NSUB:
                    ldA(n + 3)
                if n + 1 < NSUB:
                    stA(n + 1)
                if n >= 1:
                    stC(n - 1)
                if n < NSUB:
                    stB(n)
                    if n % SPB == SPB - 1 and n // SPB + 2 < NB:
                        load_w(n // SPB + 2, which=(0, 1))
                if n >= 1:
                    stD(n - 1)
                    if (n - 1) % SPB == SPB - 1 and (n - 1) // SPB + 2 < NB:
                        load_w((n - 1) // SPB + 2, which=(2,))
            cx.barrier()

        with ExitStack() as st:
            ya = [TB(st, "ya3_%d" % i, [128, 1024], BF16) for i in range(4)]
            yb = [TB(st, "yb3_%d" % i, [128, 1024], BF16) for i in range(4)]
            x1 = [TB(st, "x13_%d" % i, [128, 1024], F32) for i in range(4)]
            m_ = [TB(st, "m3_%d" % i, [128, 1024], F32) for i in range(2)]
            o_ = [TB(st, "o3_%d" % i, [128, 1024], F32) for i in range(2)]
            jk5, b_jk5 = TB(st, "jk5", [128, 1024], BF16)
            fngr, b_fngr = TB(st, "fngr3", [128, 1024], F32)
            cx.dma("sp", fngr[:], fng_rep, writes=[b_fngr])
            sq = [TB(st, "sq3_%d" % i, [128, 4], F32) for i in range(2)]

            def ld3(tt):
                rows = slice(tt * 128, (tt + 1) * 128)
                a_, b_a = ya[tt % 4]
                bb_, b_b = yb[tt % 4]
                x_, b_x = x1[tt % 4]
                cx.idma(out=a_[:], out_off=None, in_=YS, in_off=DESTi[:, tt, 0:1], reads=[b_DESTi], writes=[b_a])
                cx.idma(out=bb_[:], out_off=None, in_=YS, in_off=DESTi[:, tt, 1:2], reads=[b_DESTi], writes=[b_b])
                cx.dma("sp", x_[:], X1[rows, :], writes=[b_x])

            jk5b = [(jk5, b_jk5), TB(st, "jk5b", [128, 1024], BF16)]

            def p3_tile(tt):
                rows = slice(tt * 128, (tt + 1) * 128)
                a_, b_a = ya[tt % 4]
                bb_, b_b = yb[tt % 4]
                x_, b_x = x1[tt % 4]
                mm_, b_m = m_[tt % 2]
                oo_, b_o = o_[tt % 2]
                s_, b_s = sq[tt % 2]
                jk_, b_jk = jk5b[tt % 2]
                cx.op("act", lambda e: e.activation(out=mm_[:], in_=a_[:], func=AF.Copy, scale=WTS[:, tt, 0:1]),
                      reads=[b_a, b_WTS], writes=[b_m])
                cx.op("dve", lambda e: e.scalar_tensor_tensor(out=mm_[:], in0=bb_[:], scalar=WTS[:, tt, 1:2], in1=mm_[:],
                                                              op0=ALU.mult, op1=ALU.add), reads=[b_b, b_WTS, b_m], writes=[b_m])
                cx.op("dve", lambda e: e.tensor_tensor(out=mm_[:], in0=mm_[:], in1=x_[:], op=ALU.add), reads=[b_m, b_x], writes=[b_m])
                cx.op("act", lambda e: e.activation(out=jk_[:], in_=mm_[:], func=AF.Square, accum_out=s_[:, 0:1]),
                      reads=[b_m], writes=[b_jk, b_s])
                rstd_from_ssq(s_[:, 0:1], s_[:, 1:2], s_[:, 2:3], 1024.0, [b_s], [b_s], via="act")
                cx.op("dve", lambda e: e.scalar_tensor_tensor(out=oo_[:], in0=mm_[:], scalar=s_[:, 1:2], in1=fngr[:],
                                                              op0=ALU.mult, op1=ALU.mult), reads=[b_m, b_s, b_fngr], writes=[b_o])
                cx.dma("sp", out[rows, :], oo_[:], reads=[b_o])

            ld3(0)
            if NT > 1:
                ld3(1)
            for tt in range(0, NT, 2):
                for k in (2, 3):
                    if tt + k < NT:
                        ld3(tt + k)
                if tt + 1 < NT:
                    cx.zip_emit([lambda: p3_tile(tt), lambda: p3_tile(tt + 1)])
                else:
                    p3_tile(tt)
            cx.barrier()
    return nc


def _prep_shared(inputs, NB):
    f = lambda a: np.ascontiguousarray(np.asarray(a, dtype=np.float32))
    rep = lambda v: f(np.broadcast_to(np.asarray(v, np.float32).reshape(1, -1), (128, np.asarray(v).size)))
    col = lambda v: f(np.asarray(v, np.float32).reshape(8, 128).T)
    d = {}
    d["w_ada"] = f(inputs["w_ada"][0])
    d["b_ada"] = f(inputs["b_ada"][0].reshape(1, -1))
    d["n1g_col"] = col(inputs["norm1_g"][0])
    d["n2g_rep"] = rep(inputs["norm2_g"][0])
    d["fng_rep"] = rep(inputs["final_norm_g"])
    d["w_in"] = f(inputs["w_in"][0])
    d["lamv"] = rep(np.concatenate([np.asarray(inputs[k][0]) for k in
                                    ("attn_lambda_q1", "attn_lambda_k1", "attn_lambda_q2", "attn_lambda_k2")]))
    d["sg_rep"] = rep(inputs["attn_subln_g"][0])
    d["tb_rep"] = rep(np.asarray(inputs["rel_bias_table"]).reshape(-1))
    d["rlb_rep"] = rep(np.asarray(inputs["rec_lower_bound"]).reshape(-1))
    d["rng_rep"] = rep(inputs["rec_norm_g"][0])
    d["w_out"] = f(inputs["w_out"][0])
    wr = np.concatenate([np.asarray(inputs["w_group"][0]), np.asarray(inputs["w_expert"][0])], axis=1)
    d["wr"] = f(wr.reshape(8, 128, 36).transpose(1, 0, 2))
    d["br_rep"] = rep(np.concatenate([np.asarray(inputs["b_group"][0]), np.asarray(inputs["b_expert"][0])]))
    w1 = np.asarray(inputs["w1"][0], np.float32)
    w3 = np.asarray(inputs["w3"][0], np.float32)
    w2 = np.asarray(inputs["w2"][0], np.float32)
    d["w1r"] = f(w1.reshape(32, 8, 128, 512).transpose(0, 2, 1, 3).reshape(32 * 128, 4096))
    d["w3r"] = f(w3.reshape(32, 8, 128, 512).transpose(0, 2, 1, 3).reshape(32 * 128, 4096))
    d["w2r"] = f(w2.reshape(32, 4, 128, 1024).transpose(0, 2, 1, 3).reshape(32 * 128, 4096))
    d["consts"] = _consts(NB)
    return d


def run(inputs, S, nb, debug=False):
    NB = (2 * S) // BLK + 32
    nc = build(S, debug)
    shared = _prep_shared(inputs, NB)
    x = np.asarray(inputs["x"], np.float32)
    c = np.asarray(inputs["c"], np.float32)
    in_maps = []
    for b in range(nb):
        m = dict(shared)
        m["x"] = np.ascontiguousarray(x[b, :S])
        m["c_col"] = np.ascontiguousarray(c[b].reshape(8, 128).T)
        in_maps.append(m)
    res = run_bass_kernel_spmd(nc, in_maps, core_ids=list(range(nb)))
    return res


def kernel(**inputs):
    S = 8192
    res = run(inputs, S, 8)
    return np.stack([np.asarray(r["out"], np.float32) for r in res.results], axis=0)
```
